# Optimizing a Trainium2 kernel written in Bass

```python
import jax
import jax.numpy as jnp
from jax import lax
import numpy as np


D_MODEL = 1024
BATCH = 8
SEQ = 2048
DEPTH = 4

HEAD_DIM = 64
MOBA_HEADS = 8
NSA_HEADS = 8
NSA_KV_HEADS = 2
NSA_GROUP = NSA_HEADS // NSA_KV_HEADS
MOBA_WIDTH = MOBA_HEADS * HEAD_DIM
NSA_WIDTH = NSA_HEADS * HEAD_DIM
NSA_KV_WIDTH = NSA_KV_HEADS * HEAD_DIM
MIX_WIDTH = MOBA_WIDTH + NSA_WIDTH
NSA_BRANCHES = 3
MOBA_BLOCK = 256
MOBA_TOPK = 3
CMP_LEN = 32
CMP_STRIDE = 16
CMP_HIDDEN = 256
SLC_BLOCK = 64
SLC_TOPK = 16
WINDOW = 512
Q_BLOCK = 128
D_FF = 2816
SPLIT_WIDTHS = [MOBA_WIDTH, MOBA_WIDTH, MOBA_WIDTH, NSA_WIDTH] + [NSA_KV_WIDTH] * 6 + [NSA_BRANCHES * NSA_HEADS]
IN_WIDTH = sum(SPLIT_WIDTHS)
SPLIT_POINTS = [int(v) for v in np.cumsum(SPLIT_WIDTHS)[:-1]]
NEG = -1e30
SLC_FORCE = 1e4
EPS = 1e-6

kernel_name = 'hybrid_moba_nsa_macaron_alibi'


def rms_norm(x, g):
    xf = x.astype(jnp.float32)
    y = xf * lax.rsqrt(jnp.mean(xf * xf, axis=-1, keepdims=True) + EPS)
    return (y * g.astype(jnp.float32)).astype(x.dtype)


def swiglu(x, w_gate, w_up, w_down):
    return (jax.nn.silu(x @ w_gate) * (x @ w_up)) @ w_down


def alibi_slopes(n):
    return jnp.asarray(2.0 ** (-8.0 * np.arange(1, n + 1) / n), dtype=jnp.float32)


def masked_softmax(scores, mask):
    s = jnp.where(mask, scores.astype(jnp.float32), NEG)
    m = jnp.max(s, axis=-1, keepdims=True)
    p = jnp.where(mask, jnp.exp(s - m), 0.0)
    return p / jnp.maximum(jnp.sum(p, axis=-1, keepdims=True), 1e-30)


def take_blocks(blocks, idx):
    return jax.vmap(lambda b_, i_: b_[i_])(blocks, idx)


def batch_qblock_ids(B, S):
    nq = S // Q_BLOCK
    return jnp.repeat(jnp.arange(B), nq), jnp.tile(jnp.arange(nq), B)


def moba_attention(q, k, v, slopes):
    B, H, S, dh = q.shape
    scale = HEAD_DIM ** -0.5
    nb = -(-S // MOBA_BLOCK)
    pad = nb * MOBA_BLOCK - S
    kb_all = jnp.pad(k, ((0, 0), (0, 0), (0, pad), (0, 0))).reshape(B, H, nb, MOBA_BLOCK, dh)
    vb_all = jnp.pad(v, ((0, 0), (0, 0), (0, pad), (0, 0))).reshape(B, H, nb, MOBA_BLOCK, dh)
    k_mean = jnp.mean(kb_all.astype(jnp.float32), axis=3)
    own = jnp.arange(S) // MOBA_BLOCK
    gate = jnp.einsum('bhsd,bhnd->bhsn', q.astype(jnp.float32), k_mean)
    cand = jnp.arange(nb)[None, :] < own[:, None]
    kk = max(1, min(MOBA_TOPK, nb - 1))
    _, sel = lax.top_k(jnp.where(cand, gate, NEG), kk)
    sel_ok = sel < own[:, None]
    blk = jnp.arange(MOBA_BLOCK)
    n_sel = kk * MOBA_BLOCK

    def step(ids):
        b, qb = ids
        q0 = qb * Q_BLOCK
        t = q0 + jnp.arange(Q_BLOCK)
        qc = lax.dynamic_slice_in_dim(q[b], q0, Q_BLOCK, axis=1)
        ic = lax.dynamic_slice_in_dim(sel[b], q0, Q_BLOCK, axis=1)
        okc = lax.dynamic_slice_in_dim(sel_ok[b], q0, Q_BLOCK, axis=1)
        kb, vb = kb_all[b], vb_all[b]
        k_sel = take_blocks(kb, ic)
        v_sel = take_blocks(vb, ic)
        ob = q0 // MOBA_BLOCK
        k_own = lax.dynamic_index_in_dim(kb, ob, axis=1, keepdims=False)
        v_own = lax.dynamic_index_in_dim(vb, ob, axis=1, keepdims=False)
        s_sel = ic[..., None] * MOBA_BLOCK + blk
        s_own = ob * MOBA_BLOCK + blk
        l_sel = (jnp.einsum('hqd,hqkjd->hqkj', qc, k_sel).astype(jnp.float32) * scale
                 - slopes[:, None, None, None] * jnp.abs(t[None, :, None, None] - s_sel).astype(jnp.float32))
        l_own = (jnp.einsum('hqd,hjd->hqj', qc, k_own).astype(jnp.float32) * scale
                 - slopes[:, None, None] * jnp.abs(t[:, None] - s_own[None, :]).astype(jnp.float32)[None])
        logits = jnp.concatenate([l_sel.reshape(H, Q_BLOCK, n_sel), l_own], axis=-1)
        m_sel = jnp.broadcast_to(okc[..., None], (H, Q_BLOCK, kk, MOBA_BLOCK)).reshape(H, Q_BLOCK, n_sel)
        m_own = jnp.broadcast_to((s_own[None, :] <= t[:, None])[None], (H, Q_BLOCK, MOBA_BLOCK))
        p = masked_softmax(logits, jnp.concatenate([m_sel, m_own], axis=-1))
        p_sel = p[..., :n_sel].reshape(H, Q_BLOCK, kk, MOBA_BLOCK)
        p_own = p[..., n_sel:]
        o = jnp.einsum('hqkj,hqkjd->hqd', p_sel, v_sel) + jnp.einsum('hqj,hjd->hqd', p_own, v_own)
        return o.astype(q.dtype)

    out = lax.map(step, batch_qblock_ids(B, S))
    nq = S // Q_BLOCK
    return out.reshape(B, nq, H, Q_BLOCK, dh).transpose(0, 2, 1, 3, 4).reshape(B, H, S, dh)


def compress_blocks(kv, win_idx, pos_emb, w1, w2):
    B, G, S, dh = kv.shape
    n = win_idx.shape[0]
    blocks = kv[:, :, win_idx] + pos_emb
    return jax.nn.gelu(blocks.reshape(B, G, n, CMP_LEN * dh) @ w1) @ w2


def nsa_attention(q, k_c, v_c, k_s, v_s, k_w, v_w, gates,
                  pos_k, k_w1, k_w2, pos_v, v_w1, v_w2, slopes):
    B, G, R, S, dh = q.shape
    scale = HEAD_DIM ** -0.5
    t_all = jnp.arange(S)
    n_cmp = (S - CMP_LEN) // CMP_STRIDE + 1
    cmp_start = np.arange(n_cmp) * CMP_STRIDE
    win_idx = cmp_start[:, None] + np.arange(CMP_LEN)[None, :]
    kc = compress_blocks(k_c, win_idx, pos_k, k_w1, k_w2)
    vc = compress_blocks(v_c, win_idx, pos_v, v_w1, v_w2)
    cmp_end = jnp.asarray(cmp_start + CMP_LEN - 1)
    d_cmp = jnp.abs(t_all[:, None] - cmp_end[None, :]).astype(jnp.float32)
    l_cmp = (jnp.einsum('bgrsd,bgnd->bgrsn', q, kc).astype(jnp.float32) * scale
             - slopes[None, :, :, None, None] * d_cmp)
    p_cmp = masked_softmax(l_cmp, cmp_end[None, :] <= t_all[:, None])
    o_cmp = jnp.einsum('bgrsn,bgnd->bgrsd', p_cmp, vc)
    n_slc = S // SLC_BLOCK
    c0 = cmp_start[:, None]
    j0 = (np.arange(n_slc) * SLC_BLOCK)[None, :]
    overlap = jnp.asarray((c0 < j0 + SLC_BLOCK) & (c0 + CMP_LEN > j0), dtype=jnp.float32)
    imp = jnp.einsum('bgrsn,nj->bgsj', p_cmp, overlap)
    tb = t_all // SLC_BLOCK
    jj = jnp.arange(n_slc)[None, :]
    cand = jj <= tb[:, None]
    forced = (jj == 0) | (jj == tb[:, None]) | (jj == tb[:, None] - 1)
    score = jnp.where(cand, jnp.where(forced, SLC_FORCE, imp), NEG)
    k_top = min(SLC_TOPK, n_slc)
    _, sel = lax.top_k(score, k_top)
    sel_ok = sel <= tb[:, None]
    ksb = k_s.reshape(B, G, n_slc, SLC_BLOCK, dh)
    vsb = v_s.reshape(B, G, n_slc, SLC_BLOCK, dh)
    kw_pad = jnp.pad(k_w, ((0, 0), (0, 0), (WINDOW, 0), (0, 0)))
    vw_pad = jnp.pad(v_w, ((0, 0), (0, 0), (WINDOW, 0), (0, 0)))
    blk = jnp.arange(SLC_BLOCK)
    n_sel = k_top * SLC_BLOCK

    def step(ids):
        b, qb = ids
        q0 = qb * Q_BLOCK
        t = q0 + jnp.arange(Q_BLOCK)
        qc = lax.dynamic_slice_in_dim(q[b], q0, Q_BLOCK, axis=2)
        ic = lax.dynamic_slice_in_dim(sel[b], q0, Q_BLOCK, axis=1)
        okc = lax.dynamic_slice_in_dim(sel_ok[b], q0, Q_BLOCK, axis=1)
        k_sel = take_blocks(ksb[b], ic).reshape(G, Q_BLOCK, n_sel, dh)
        v_sel = take_blocks(vsb[b], ic).reshape(G, Q_BLOCK, n_sel, dh)
        dist = (t[None, :, None, None] - (ic[..., None] * SLC_BLOCK + blk)).reshape(G, Q_BLOCK, n_sel)
        l_s = (jnp.einsum('grqd,gqnd->grqn', qc, k_sel).astype(jnp.float32) * scale
               - slopes[:, :, None, None] * jnp.abs(dist).astype(jnp.float32)[:, None])
        m_s = jnp.broadcast_to(okc[..., None], (G, Q_BLOCK, k_top, SLC_BLOCK)).reshape(G, Q_BLOCK, n_sel) & (dist >= 0)
        p_s = masked_softmax(l_s, m_s[:, None])
        o_s = jnp.einsum('grqn,gqnd->grqd', p_s, v_sel)
        kwc = lax.dynamic_slice_in_dim(kw_pad[b], q0, Q_BLOCK + WINDOW, axis=1)
        vwc = lax.dynamic_slice_in_dim(vw_pad[b], q0, Q_BLOCK + WINDOW, axis=1)
        s_w = q0 - WINDOW + jnp.arange(Q_BLOCK + WINDOW)
        dw = t[:, None] - s_w[None, :]
        m_w = (dw >= 0) & (dw < WINDOW) & (s_w[None, :] >= 0)
        l_w = (jnp.einsum('grqd,gjd->grqj', qc, kwc).astype(jnp.float32) * scale
               - slopes[:, :, None, None] * jnp.abs(dw).astype(jnp.float32)[None, None])
        p_w = masked_softmax(l_w, m_w)
        o_w = jnp.einsum('grqj,gjd->grqd', p_w, vwc)
        return o_s.astype(q.dtype), o_w.astype(q.dtype)

    o_s, o_w = lax.map(step, batch_qblock_ids(B, S))
    nq = S // Q_BLOCK
    o_s = o_s.reshape(B, nq, G, R, Q_BLOCK, dh).transpose(0, 2, 3, 1, 4, 5).reshape(B, G, R, S, dh)
    o_w = o_w.reshape(B, nq, G, R, Q_BLOCK, dh).transpose(0, 2, 3, 1, 4, 5).reshape(B, G, R, S, dh)
    o = gates[..., 0:1] * o_cmp + gates[..., 1:2] * o_s + gates[..., 2:3] * o_w
    return o.astype(q.dtype)


def hybrid_mixer(h, w_in, cmp_pos_k, cmp_k_w1, cmp_k_w2, cmp_pos_v, cmp_v_w1, cmp_v_w2,
                 moba_out_norm, nsa_out_norm, w_out):
    B, S, _ = h.shape
    proj = h @ w_in
    q_m, k_m, v_m, q_n, k_c, v_c, k_s, v_s, k_w, v_w, g = jnp.split(proj, SPLIT_POINTS, axis=-1)

    def heads(z, n):
        return z.reshape(B, S, n, HEAD_DIM).transpose(0, 2, 1, 3)

    o_m = moba_attention(heads(q_m, MOBA_HEADS), heads(k_m, MOBA_HEADS), heads(v_m, MOBA_HEADS),
                         alibi_slopes(MOBA_HEADS))
    qn = heads(q_n, NSA_HEADS).reshape(B, NSA_KV_HEADS, NSA_GROUP, S, HEAD_DIM)
    gates = jax.nn.sigmoid(g.astype(jnp.float32)).reshape(B, S, NSA_KV_HEADS, NSA_GROUP, NSA_BRANCHES).transpose(0, 2, 3, 1, 4)
    o_n = nsa_attention(qn, heads(k_c, NSA_KV_HEADS), heads(v_c, NSA_KV_HEADS),
                        heads(k_s, NSA_KV_HEADS), heads(v_s, NSA_KV_HEADS),
                        heads(k_w, NSA_KV_HEADS), heads(v_w, NSA_KV_HEADS), gates,
                        cmp_pos_k, cmp_k_w1, cmp_k_w2, cmp_pos_v, cmp_v_w1, cmp_v_w2,
                        alibi_slopes(NSA_HEADS).reshape(NSA_KV_HEADS, NSA_GROUP))
    o_m = o_m.transpose(0, 2, 1, 3).reshape(B, S, MOBA_WIDTH)
    o_n = o_n.transpose(0, 3, 1, 2, 4).reshape(B, S, NSA_WIDTH)
    y = jnp.concatenate([rms_norm(o_m, moba_out_norm), rms_norm(o_n, nsa_out_norm)], axis=-1)
    return y @ w_out


def setup_inputs(seed: int = 0) -> dict:
    key = jax.random.key(seed)
    ks = jax.random.split(key, 22)
    f32 = jnp.float32
    L = DEPTH

    def w(k, shape, fan_in):
        return jax.random.normal(k, shape, f32) * (fan_in ** -0.5)

    def gain(k, shape):
        return 1.0 + 0.02 * jax.random.normal(k, shape, f32)

    return {
        'x': jax.random.normal(ks[0], (BATCH, SEQ, D_MODEL), f32),
        'ffa_norm': gain(ks[1], (L, D_MODEL)),
        'ffa_w_gate': w(ks[2], (L, D_MODEL, D_FF), D_MODEL),
        'ffa_w_up': w(ks[3], (L, D_MODEL, D_FF), D_MODEL),
        'ffa_w_down': w(ks[4], (L, D_FF, D_MODEL), D_FF),
        'mix_norm': gain(ks[5], (L, D_MODEL)),
        'w_in': w(ks[6], (L, D_MODEL, IN_WIDTH), D_MODEL),
        'cmp_pos_k': 0.1 * jax.random.normal(ks[7], (L, CMP_LEN, HEAD_DIM), f32),
        'cmp_k_w1': w(ks[8], (L, CMP_LEN * HEAD_DIM, CMP_HIDDEN), CMP_LEN * HEAD_DIM),
        'cmp_k_w2': w(ks[9], (L, CMP_HIDDEN, HEAD_DIM), CMP_HIDDEN),
        'cmp_pos_v': 0.1 * jax.random.normal(ks[10], (L, CMP_LEN, HEAD_DIM), f32),
        'cmp_v_w1': w(ks[11], (L, CMP_LEN * HEAD_DIM, CMP_HIDDEN), CMP_LEN * HEAD_DIM),
        'cmp_v_w2': w(ks[12], (L, CMP_HIDDEN, HEAD_DIM), CMP_HIDDEN),
        'moba_out_norm': gain(ks[13], (L, MOBA_WIDTH)),
        'nsa_out_norm': gain(ks[14], (L, NSA_WIDTH)),
        'w_out': w(ks[15], (L, MIX_WIDTH, D_MODEL), MIX_WIDTH),
        'ffb_norm': gain(ks[16], (L, D_MODEL)),
        'ffb_w_gate': w(ks[17], (L, D_MODEL, D_FF), D_MODEL),
        'ffb_w_up': w(ks[18], (L, D_MODEL, D_FF), D_MODEL),
        'ffb_w_down': w(ks[19], (L, D_FF, D_MODEL), D_FF),
        'final_norm': gain(ks[20], (D_MODEL,)),
    }


def reference(x, ffa_norm, ffa_w_gate, ffa_w_up, ffa_w_down, mix_norm, w_in,
              cmp_pos_k, cmp_k_w1, cmp_k_w2, cmp_pos_v, cmp_v_w1, cmp_v_w2,
              moba_out_norm, nsa_out_norm, w_out,
              ffb_norm, ffb_w_gate, ffb_w_up, ffb_w_down, final_norm):
    for l in range(DEPTH):
        x = x + 0.5 * swiglu(rms_norm(x, ffa_norm[l]), ffa_w_gate[l], ffa_w_up[l], ffa_w_down[l])
        x = x + hybrid_mixer(rms_norm(x, mix_norm[l]), w_in[l],
                             cmp_pos_k[l], cmp_k_w1[l], cmp_k_w2[l],
                             cmp_pos_v[l], cmp_v_w1[l], cmp_v_w2[l],
                             moba_out_norm[l], nsa_out_norm[l], w_out[l])
        x = x + 0.5 * swiglu(rms_norm(x, ffb_norm[l]), ffb_w_gate[l], ffb_w_up[l], ffb_w_down[l])
    return rms_norm(x, final_norm)
```

```python
import bisect
from contextlib import ExitStack

import numpy as np
import ml_dtypes

import concourse.bass as bass
import concourse.mybir as mybir
from concourse.bass_utils import run_bass_kernel_spmd

F32 = mybir.dt.float32
BF16 = mybir.dt.bfloat16
AF = mybir.ActivationFunctionType
ALU = mybir.AluOpType
AX = mybir.AxisListType

S = 2048
D = 1024
DFF = 2816
NF = 22
NT = 16
NCK = 4
L_FULL = 4
EPS = 1e-6
BIG = 32768.0
NCMP = 127

ENGS = ("pe", "act", "dve", "pool", "sp")


class Sched:
    def __init__(self):
        self.ops = {e: [] for e in ENGS}
        self.known = {e: {} for e in ENGS}
        self.hist = {e: [(-1, {})] for e in ENGS}
        self.hist_idx = {e: [-1] for e in ENGS}
        self.marks = {e: [] for e in ENGS}
        self.lastw = {}
        self.readers = {}
        self.chan_cnt = {}
        self.last_compute = {e: -1 for e in ENGS}

    def _snapshot(self, eng):
        idx = len(self.ops[eng])
        self.hist[eng].append((idx, dict(self.known[eng])))
        self.hist_idx[eng].append(idx)

    def _clock(self, eng, idx):
        pos = bisect.bisect_right(self.hist_idx[eng], idx) - 1
        return self.hist[eng][pos][1]

    def _need(self, eng, src, idx, waits):
        kn = self.known[eng]
        if isinstance(src, tuple):
            if kn.get(src, 0) >= idx:
                return False
            tot = self.chan_cnt[src[1]]
            waits.append((src, tot * 16))
            kn[src] = tot
            return True
        if kn.get(src, -1) >= idx:
            return False
        mk = self.marks[src]
        pos = bisect.bisect_left(mk, idx)
        if pos == len(mk):
            mk.append(idx)
            self.ops[src][idx][2] = True
        midx = mk[pos]
        waits.append((src, pos + 1))
        kn[src] = midx
        for s2, v2 in self._clock(src, midx).items():
            if isinstance(s2, tuple):
                if kn.get(s2, 0) < v2:
                    kn[s2] = v2
            elif kn.get(s2, -1) < v2:
                kn[s2] = v2
        return True

    def add(self, eng, fn, r=(), w=(), ch=None):
        deps = []
        lastw = self.lastw
        readers = self.readers
        for b in r:
            d = lastw.get(b)
            if d is not None:
                deps.append(d)
        skip_same = (eng == "pe")
        for b in w:
            d = lastw.get(b)
            if d is not None and not (skip_same and d[0] == eng):
                deps.append(d)
            for d in readers.get(b, ()):
                if not (skip_same and d[0] == eng):
                    deps.append(d)
        waits = []
        changed = False
        for (src, idx) in deps:
            if self._need(eng, src, idx, waits):
                changed = True
        idx = len(self.ops[eng])
        if changed:
            self._snapshot(eng)
        self.ops[eng].append([fn, waits, False])
        if ch is not None:
            self.chan_cnt[ch] = self.chan_cnt.get(ch, 0) + 1
            me = (("ch", ch), self.chan_cnt[ch])
            self.ops[eng][idx][2] = ("ch", ch)
        else:
            me = (eng, idx)
            self.last_compute[eng] = idx
        for b in r:
            readers.setdefault(b, []).append(me)
        for b in w:
            lastw[b] = me
            readers[b] = []
        return idx

    def barrier(self):
        for e in ENGS:
            waits = []
            ch = False
            for f in ENGS:
                if f != e and self.last_compute[f] >= 0:
                    ch |= self._need(e, f, self.last_compute[f], waits)
            for c, n in self.chan_cnt.items():
                ch |= self._need(e, ("ch", c), n, waits)
            if ch:
                self._snapshot(e)
            self.ops[e].append([None, waits, False])
        self.lastw = {}
        self.readers = {}


def build(L=L_FULL, final=True, stages=("ffa", "mix", "ffb"), dbg=(), mix_parts=("moba", "nsa", "out")):
    nc = bass.Bass("TRN2", target_bir_lowering=False)
    sc = Sched()
    es = ExitStack()

    def din(name, shape, dt=F32):
        return nc.dram_tensor(name, list(shape), dt, kind="ExternalInput").ap()

    xT_d = din("xT", [D, S])
    gains_d = din("gains", [128, 13, 8])
    wgu_d = din("wgu", [L_FULL, 2, NF, 128, 2, 8, 128])
    wd_d = din("wd", [L_FULL, 2, 2, 8, 128, 11, 128])
    winf_d = din("winf", [L_FULL, 16, 128, 8, 128])
    winvm_d = din("winvm", [L_FULL, 4, 128, 8, 128])
    winvn_d = din("winvn", [L_FULL, 128, 8, 280])
    wout_d = din("wout", [L_FULL, 8, 128, 8, 128])
    cw1_d = din("cw1", [L_FULL, 128, 32, 256])
    cw2_d = din("cw2", [L_FULL, 128, 2, 2, 64])
    cpos_d = din("cpos", [L_FULL, 128, 32])
    onorm_d = din("onorm", [L_FULL, 128, 1024])
    c_ident_d = din("c_ident", [128, 128], BF16)
    c_tri_d = din("c_tri", [128, 2, 128], BF16)
    c_cmpmask_d = din("c_cmpmask", [128, S], BF16)
    c_qalibi_d = din("c_qalibi", [8, 4, S], BF16)
    c_kalibi_d = din("c_kalibi", [4, S], BF16)
    c_kalibic_d = din("c_kalibic", [4, 128], BF16)
    c_emoba_d = din("c_emoba", [32, S], BF16)
    c_eslc_d = din("c_eslc", [32, S], BF16)
    c_force_d = din("c_force", [128, 8, 32])
    c_mobaneg_d = din("c_mobaneg", [128, 8, 8])
    c_vcaug_d = din("c_vcaug", [128, 33], BF16)
    outT_d = nc.dram_tensor("outT", [D, S], F32, kind="ExternalOutput").ap()
    dbg_d = {}

    DTB = {F32: 4, BF16: 2}
    SB_BASE = 16544
    alloc_state = {"off": SB_BASE, "max": 0}

    def sb(name, shape, dt):
        n = 1
        for s_ in shape[1:]:
            n *= s_
        nbytes = (n * DTB[dt] + 31) // 32 * 32
        off = alloc_state["off"]
        t = nc.alloc_sbuf_tensor_at(name, list(shape), dt, offset=off)
        alloc_state["off"] = off + nbytes
        alloc_state["max"] = max(alloc_state["max"], off + nbytes)
        return t

    xT = sb("xT", [128, 8, S], F32)
    hT = sb("hT", [128, 8, S], BF16)
    NA_OALL = NT * 1024
    NA_VA = 2 * NT * 130
    OFF_VA = NA_OALL
    OFF_QB = OFF_VA + NA_VA
    OFF_KB = OFF_QB + 4 * S
    NA = OFF_KB + 2 * S
    assert NA >= 11 * S
    arena = sb("arena", [128, NA], BF16)
    gains = sb("gains_sb", [128, 13, 8], F32)
    ident = sb("ident", [128, 128], BF16)
    tri = sb("tri", [128, 2, 128], BF16)
    cmpmask = sb("cmpmask", [128, S], BF16)
    ones = sb("ones", [128, 128], BF16)
    zeros = sb("zeros", [128, 512], BF16)
    force = sb("force", [128, 8, 32], F32)
    mobaneg = sb("mobaneg", [128, 8, 8], F32)
    epsb = sb("epsb", [128, 1], F32)
    kcT = [sb(f"kcT{g}", [128, 128], BF16) for g in range(2)]
    vcaug = [sb(f"vcaug{g}", [128, 97], BF16) for g in range(2)]
    ksumf = sb("ksumf", [64, 2, 8], F32)
    ksumb = sb("ksumb", [64, 2, 8], BF16)
    gsb = sb("gsb", [128, 2, 8, 8], F32)
    top8 = sb("top8", [128, 16, 8], F32)
    mbtok = sb("mbtok", [128, 2, 8, 32], BF16)
    rs = [sb(f"rs{i}", [128, 4], F32) for i in range(4)]
    rinv = [sb(f"rinv{i}", [128, 4], F32) for i in range(4)]
    coef = [sb(f"coef{i}", [128, 4], F32) for i in range(4)]
    gates = sb("gates", [128, NT, 24], F32)
    imp = sb("imp", [128, 4, 32], F32)
    impraw = sb("impraw", [128, 4, 32], F32)
    imptmp = sb("imptmp", [128, 4, 32], F32)
    imp3 = sb("imp3", [128, 4, 32], F32)
    imp4 = [imp, impraw, imptmp, imp3]
    score = sb("score", [128, 4, 32], F32)
    score2 = sb("score2", [128, 4, 32], F32)
    t8a = sb("t8a", [128, 4, 8], F32)
    t8b = sb("t8b", [128, 4, 8], F32)
    mbn = sb("mbn", [128, 4, 32], BF16)
    posb = sb("posb", [128, 2], F32)
    ssq = sb("ssq", [128, 2], F32)
    srt = sb("srt", [128, 2], F32)
    off_n = alloc_state["off"]
    sq = [sb("sq0", [128, 8, 256], BF16)]
    rstd = [sb(f"rstd{i}", [128, 256], F32) for i in range(2)]
    end_n = alloc_state["off"]
    alloc_state["off"] = off_n
    gx = sb("gx", [128, 508], F32)
    gu = sb("gu", [128, 508], F32)
    hid = sb("hid", [128, 508], BF16)
    assert alloc_state["off"] <= end_n, (alloc_state["off"], end_n)
    alloc_state["off"] = end_n
    off_u = alloc_state["off"]
    WGU_SLOTS = 2
    wgu = [sb(f"wgu{i}", [128, 2, 8, 128], BF16) for i in range(WGU_SLOTS)]
    WD_SLOTS = 2
    wdw = [sb(f"wdw{i}", [128, 11, 128], BF16) for i in range(WD_SLOTS)]
    sg = [sb(f"sg{i}", [128, 512], BF16) for i in range(2)]
    outbuf = [sb(f"outbuf{i}", [128, 256], F32) for i in range(2)]
    alloc_state["off"] = off_u
    WF_SLOTS = 2
    wf = [sb(f"wf{i}", [128, 8, 128], BF16) for i in range(WF_SLOTS)]
    onorm = sb("onorm_sb", [128, 1024], F32)
    w2s = sb("w2s", [128, 2, 2, 64], BF16)
    cpos = sb("cpos_sb", [128, 32], BF16)
    pt = [sb(f"pt{i}", [128, 512], BF16) for i in range(2)]
    oacc = sb("oacc", [128, 4, 256], F32)
    off_a = alloc_state["off"]
    w1s = [sb(f"w1s{i}", [128, 4, 256], BF16) for i in range(2)]
    wvn = sb("wvn", [128, 8, 280], BF16)
    end_a = alloc_state["off"]
    alloc_state["off"] = off_a
    ytok = [sb(f"ytok{i}", [128, 1024], BF16) for i in range(1)]
    yT = sb("yT", [128, 8, 512], BF16)
    alloc_state["off"] = max(alloc_state["off"], end_a)
    assert alloc_state["max"] <= 229376, alloc_state["max"]

    ps = [es.enter_context(nc.psum_tensor(f"ps{i}", [128, 512], F32)) for i in range(7)]
    psb = es.enter_context(nc.psum_tensor("psb", [128, 1024], BF16))

    def PS(i):
        return ("ps", i)

    def oall(tile, a, b):
        return arena[:, tile * 1024 + a: tile * 1024 + b]

    def QB(i):
        return arena[:, OFF_QB + i * S: OFF_QB + (i + 1) * S]

    def KB(i):
        return arena[:, OFF_KB + i * S: OFF_KB + (i + 1) * S]

    def VA(i):
        return arena[:, OFF_VA + i * NT * 130: OFF_VA + (i + 1) * NT * 130].rearrange(
            "p (t h c) -> p t h c", h=2, c=65)

    def CIN(g):
        return arena[:, OFF_VA + g * S: OFF_VA + (g + 1) * S]

    def AT(fi):
        return arena[:, fi * S:(fi + 1) * S]

    add = sc.add
    rot = {}

    def nxt(name, n):
        v = rot.get(name, 0)
        rot[name] = v + 1
        return v % n

    def dma_sp(out, in_, ch, r=(), w=()):
        add("sp", lambda e, o=out, i=in_: e.dma_start(out=o, in_=i), r=r, w=w, ch=ch)

    def dma_cast(out, in_, ch, r=(), w=()):
        add("pool", lambda e, o=out, i=in_: e.dma_start(out=o, in_=i), r=r, w=w, ch=ch)

    for c in range(8):
        dma_sp(xT[:, c, :], xT_d[c * 128:(c + 1) * 128, :], "x", w=[("xT", c, k) for k in range(NCK)])
    dma_sp(gains[:], gains_d[:, :, :], "c0", w=["gains"])
    dma_sp(ident[:], c_ident_d[:, :], "c0", w=["ident"])
    dma_sp(tri[:], c_tri_d[:, :, :], "c0", w=["tri"])
    dma_sp(cmpmask[:], c_cmpmask_d[:, :], "c0", w=["cmpmask"])
    dma_sp(force[:], c_force_d[:, :, :], "c0", w=["force"])
    dma_sp(mobaneg[:], c_mobaneg_d[:, :, :], "c0", w=["mobaneg"])
    for g in range(2):
        dma_sp(kcT[g][64:68, :], c_kalibic_d[:, :], "c0", w=[("kcT", g, "al")])
        dma_sp(vcaug[g][:, 64:97], c_vcaug_d[:, :], "c0", w=[("vcaug", g, "c")])
    add("dve", lambda e: e.memset(ones[:], 1.0), w=["ones"])
    add("dve", lambda e: e.memset(zeros[:], 0.0), w=["zeros"])
    add("dve", lambda e: e.memset(epsb[:], EPS), w=["epsb"])
    add("dve", lambda e: e.memset(mbtok[:], 0.0), w=["mbtok"])

    if dbg:
        add("pool", lambda e: e.memset(arena[:, 0:NA_OALL], 0.0),
            w=[("oall", t, h) for t in range(NT) for h in range(16)])

    def norm_fm(gidx, final_out=False):
        for hc in range(8):
            tc = hc // 2
            cs = slice(hc * 256, (hc + 1) * 256)
            xk = [("xT", c, tc) for c in range(8)]
            add("act", lambda e, cs=cs: e.activation(out=sq[0][:], in_=xT[:, :, cs], func=AF.Square),
                r=xk, w=[("sq", 0)])
            for c in range(8):
                add("pe", lambda e, c=c: e.matmul(ps[6][:, 0:256], ones[:, :], sq[0][:, c, :],
                                                  start=(c == 0), stop=(c == 7)),
                    r=[("sq", 0), "ones"], w=[PS(6)])
            b = nxt("rstd", 2)
            add("act", lambda e, b=b: e.activation(out=rstd[b][:], in_=ps[6][:, 0:256], func=AF.Sqrt,
                                                   bias=epsb[:, 0:1], scale=1.0 / D),
                r=[PS(6), "epsb"], w=[("rstd", b)])
            add("dve", lambda e, b=b: e.reciprocal(out=rstd[b][:], in_=rstd[b][:]),
                r=[("rstd", b)], w=[("rstd", b)])
            for c in range(8):
                if not final_out:
                    add("dve", lambda e, b=b, c=c, cs=cs: e.scalar_tensor_tensor(
                        out=hT[:, c, cs], in0=xT[:, c, cs], scalar=gains[:, gidx, c:c + 1], in1=rstd[b][:],
                        op0=ALU.mult, op1=ALU.mult),
                        r=[("xT", c, tc), ("rstd", b), "gains"], w=[("hT", c, tc)])
                else:
                    ob = nxt("outbuf", 2)
                    add("dve", lambda e, b=b, c=c, cs=cs, ob=ob: e.scalar_tensor_tensor(
                        out=outbuf[ob][:], in0=xT[:, c, cs], scalar=gains[:, gidx, c:c + 1], in1=rstd[b][:],
                        op0=ALU.mult, op1=ALU.mult),
                        r=[("xT", c, tc), ("rstd", b), "gains"], w=[("outbuf", ob)])
                    dma_sp(outT_d[c * 128:(c + 1) * 128, cs], outbuf[ob][:], "out", r=[("outbuf", ob)])

    def ffn(l, ab):
        norm_fm(3 * l + (0 if ab == 0 else 2))
        for half in range(2):
            for fi in range(11):
                f = half * 11 + fi
                s_ = nxt("wgu", WGU_SLOTS)
                for gu_ in range(2):
                    dma_cast(wgu[s_][:, gu_, :, :], wgu_d[l, ab, f, :, gu_, :, :], f"wgu{s_}", w=[("wgu", s_)])
                for tc in range(NCK):
                    cs = slice(tc * 512, (tc + 1) * 512)
                    pb = nxt("ffn_ps", 2)
                    G, U = ps[pb], ps[2 + pb]
                    for gu_, P in ((0, G), (1, U)):
                        for c in range(8):
                            add("pe", lambda e, P=P, s_=s_, gu_=gu_, c=c, cs=cs: e.matmul(
                                P[:, :], wgu[s_][:, gu_, c, :], hT[:, c, cs], start=(c == 0), stop=(c == 7)),
                                r=[("wgu", s_), ("hT", c, tc)], w=[PS(pb + 2 * gu_)])
                    sb_ = nxt("sg", 2)
                    add("act", lambda e, G=G, sb_=sb_: e.activation(out=sg[sb_][:], in_=G[:, :], func=AF.Silu),
                        r=[PS(pb)], w=[("sg", sb_)])
                    add("dve", lambda e, U=U, sb_=sb_, fi=fi, cs=cs: e.tensor_tensor(
                        out=AT(fi)[:, cs], in0=U[:, :], in1=sg[sb_][:], op=ALU.mult),
                        r=[PS(2 + pb), ("sg", sb_)], w=[("AT", fi, tc)])
            for dc in range(8):
                s_ = nxt("wd", WD_SLOTS)
                dma_cast(wdw[s_][:], wd_d[l, ab, half, dc, :, :, :], f"wd{s_}", w=[("wd", s_)])
                for tc in range(NCK):
                    cs = slice(tc * 512, (tc + 1) * 512)
                    pb = 4 + nxt("ffn_pd", 2)
                    for k in range(11):
                        add("pe", lambda e, pb=pb, s_=s_, k=k, cs=cs: e.matmul(
                            ps[pb][:, :], wdw[s_][:, k, :], AT(k)[:, cs], start=(k == 0), stop=(k == 10)),
                            r=[("wd", s_), ("AT", k, tc)], w=[PS(pb)])
                    add("dve", lambda e, pb=pb, dc=dc, cs=cs: e.scalar_tensor_tensor(
                        out=xT[:, dc, cs], in0=ps[pb][:, :], scalar=0.5, in1=xT[:, dc, cs],
                        op0=ALU.mult, op1=ALU.add),
                        r=[PS(pb), ("xT", dc, tc)], w=[("xT", dc, tc)])

    def load_wf(l, blk):
        s_ = nxt("wf", WF_SLOTS)
        dma_cast(wf[s_][:], winf_d[l, blk, :, :, :], f"wf{s_}", w=[("wf", s_)])
        return s_

    def proj_fm(s_, evac):
        for tc in range(NCK):
            cs = slice(tc * 512, (tc + 1) * 512)
            pb = nxt("prj", 2)
            for c in range(8):
                add("pe", lambda e, pb=pb, s_=s_, c=c, cs=cs: e.matmul(
                    ps[pb][:, :], wf[s_][:, c, :], hT[:, c, cs], start=(c == 0), stop=(c == 7)),
                    r=[("wf", s_), ("hT", c, tc)], w=[PS(pb)])
            evac(tc, ps[pb], pb)

    def copy_evac(eng, out, in_, r, w):
        if eng == "act":
            add("act", lambda e: e.activation(out=out, in_=in_, func=AF.Copy), r=r, w=w)
        else:
            add(eng, lambda e: e.tensor_copy(out, in_), r=r, w=w)

    def attn_chunk(c, Qap, qkey, Kap, kkey, krows, vfn, vkey, kts, special, obank, mask_in_q):
        tiles = list(range(4 * c, 4 * c + 4))
        O = ps[obank]
        add("pe", lambda e: e.matmul(O[:, 0:260], zeros[:, 0:128], zeros[:, 0:260], start=True, stop=False),
            r=["zeros"], w=[PS(obank)])
        alljs = sorted(set(j for i in tiles for j in kts(i)))
        lastj = {i: max(kts(i)) for i in tiles}
        qreads = [(qkey, "q", c), (qkey, "al"), "qkinit"]
        if mask_in_q:
            qreads.append((qkey, "m", c))
        for j in alljs:
            iset = [i for i in tiles if j in kts(i)]
            i_lo, i_hi = min(iset), max(iset)
            N = (i_hi - i_lo + 1) * 128
            sbk = 2 + nxt("st", 2)
            ST = ps[sbk]
            specs = [(i, special(j, i)) for i in iset if special(j, i) is not None]
            add("pe", lambda e, ST=ST, j=j, i_lo=i_lo, i_hi=i_hi, N=N, ns=len(specs): e.matmul(
                ST[:, 0:N], Kap[0:krows, j * 128:(j + 1) * 128], Qap[0:krows, i_lo * 128:(i_hi + 1) * 128],
                start=True, stop=(ns == 0)),
                r=qreads + [(kkey, "k", j // 4), (kkey, "al"), (kkey, "E")], w=[PS(sbk)])
            for si, (i, kind) in enumerate(specs):
                add("pe", lambda e, ST=ST, i=i, i_lo=i_lo, kind=kind, last=(si == len(specs) - 1): e.matmul(
                    ST[:, (i - i_lo) * 128:(i - i_lo + 1) * 128], ident[:, :], tri[:, kind, :],
                    start=False, stop=last),
                    r=["ident", "tri"], w=[PS(sbk)])
            pb_ = nxt("pt", 2)
            add("act", lambda e, ST=ST, N=N, pb_=pb_: e.activation(
                out=pt[pb_][:, 0:N], in_=ST[:, 0:N], func=AF.Exp, scale=0.125),
                r=[PS(sbk)], w=[("pt", pb_)])
            for i in iset:
                q = i - 4 * c
                add("pe", lambda e, pb_=pb_, i=i, i_lo=i_lo, q=q, j=j, last=(j == lastj[i] and i == tiles[-1]):
                    e.matmul(O[:, q * 65:(q + 1) * 65], pt[pb_][:, (i - i_lo) * 128:(i - i_lo + 1) * 128],
                             vfn(j), start=False, stop=last),
                    r=[("pt", pb_), (vkey, j // 4)], w=[PS(obank)])

    def rinv_of(obank, stride, col, kbuf):
        O = ps[obank]
        add("dve", lambda e: e.tensor_scalar(
            out=rs[kbuf][:, :], in0=O[:, 0:4 * stride].rearrange("p (q c) -> p q c", c=stride)[:, :, col],
            scalar1=1e-30, scalar2=None, op0=ALU.max),
            r=[PS(obank)], w=[("rs", kbuf)])
        add("dve", lambda e: e.reciprocal(out=rinv[kbuf][:, :], in_=rs[kbuf][:, :]),
            r=[("rs", kbuf)], w=[("rinv", kbuf)])

    def mixer(l):
        norm_fm(3 * l + 1)
        dma_sp(onorm[:], onorm_d[l, :, :], "onorm", w=["onorm"])
        add("pool", lambda e: e.memset(arena[:, OFF_QB:NA], 0.0), w=["qkinit"])
        for i in range(2):
            dma_sp(KB(i)[64:68, :], c_kalibi_d[:, :], f"kbal{i}", r=["qkinit"], w=[(("KB", i), "al")])
        for i in range(2):
            dma_sp(KB(i)[96:128, :], c_emoba_d[:, :], f"kbE{i}", r=["qkinit"], w=[(("KB", i), "E")])
        for i in range(2):
            add("pool", lambda e, i=i: e.memset(VA(i)[:, :, :, 64:65], 1.0), w=[("VAone", i)])

        import os
        KSTOP = os.environ.get("KSTOP", "")
        if KSTOP == "init":
            return
        if "moba" in mix_parts:
            for p in range(4 if not KSTOP else 1):
                qb = [0, 1]
                va = p % 2
                for e_ in range(2):
                    h = 2 * p + e_
                    dma_sp(QB(qb[e_])[64:68, :], c_qalibi_d[h, :, :], f"qal{qb[e_]}", r=["qkinit"],
                           w=[(("QB", qb[e_]), "al")])
                s_ = load_wf(l, p)

                def evq(tc, P, pb, qb=qb):
                    cs = slice(tc * 512, (tc + 1) * 512)
                    copy_evac("act", QB(qb[0])[0:64, cs], P[0:64, :], [PS(pb), "qkinit"], [(("QB", qb[0]), "q", tc)])
                    copy_evac("dve", QB(qb[1])[0:64, cs], P[64:128, :], [PS(pb), "qkinit"], [(("QB", qb[1]), "q", tc)])
                proj_fm(s_, evq)
                s_ = load_wf(l, 4 + p)

                def evk(tc, P, pb, qb=qb):
                    for e_ in range(2):
                        for hf in range(2):
                            c0 = tc * 512 + hf * 256
                            add("act", lambda e, e_=e_, hf=hf, c0=c0, P=P, tc=tc: e.activation(
                                out=KB(qb[e_])[0:64, c0:c0 + 256], in_=P[64 * e_:64 * e_ + 64, hf * 256:hf * 256 + 256],
                                func=AF.Copy, accum_out=ksumf[0:64, e_, 2 * tc + hf:2 * tc + hf + 1]),
                                r=[PS(pb), "qkinit"], w=[(("KB", qb[e_]), "k", tc), ("ksumf", e_, tc)])
                proj_fm(s_, evk)
                add("dve", lambda e: e.tensor_copy(ksumb[:, :, :], ksumf[:, :, :]),
                    r=[("ksumf", e_, tc) for e_ in range(2) for tc in range(NCK)], w=["ksumb"])
                if KSTOP == "qk":
                    return
                s_ = nxt("wf", WF_SLOTS)
                dma_cast(wf[s_][:], winvm_d[l, p, :, :, :], f"wf{s_}", w=[("wf", s_)])
                for tq in range(4):
                    pb = nxt("prj", 2)
                    for ti in range(4):
                        t = tq * 4 + ti
                        for c in range(8):
                            add("pe", lambda e, pb=pb, s_=s_, c=c, t=t, ti=ti: e.matmul(
                                ps[pb][:, ti * 128:(ti + 1) * 128], hT[:, c, t * 128:(t + 1) * 128], wf[s_][:, c, :],
                                start=(c == 0), stop=(c == 7)),
                                r=[("wf", s_), ("hT", c, t // 4)], w=[PS(pb)])
                    add("dve", lambda e, pb=pb, tq=tq, va=va: e.tensor_copy(
                        VA(va)[:, 4 * tq:4 * tq + 4, :, 0:64],
                        ps[pb][:, :].rearrange("p (t h c) -> p t h c", h=2, c=64)),
                        r=[PS(pb)], w=[(("VA", va), tq)])
                if KSTOP == "v":
                    return
                for e_ in range(2):
                    for ti in range(8):
                        t = 8 + ti
                        add("pe", lambda e, e_=e_, ti=ti, t=t, qb=qb: e.matmul(
                            ps[6][:, (e_ * 8 + ti) * 8:(e_ * 8 + ti) * 8 + 8],
                            QB(qb[e_])[0:64, t * 128:(t + 1) * 128], ksumb[0:64, e_, :], start=True, stop=True),
                            r=[(("QB", qb[e_]), "q", t // 4), "ksumb"], w=[PS(6)])
                for e_ in range(2):
                    add("dve", lambda e, e_=e_: e.tensor_tensor(
                        out=gsb[:, e_, :, :], in0=ps[6][:, e_ * 64:(e_ + 1) * 64].rearrange("p (t n) -> p t n", n=8),
                        in1=mobaneg[:, :, :], op=ALU.add),
                        r=[PS(6), "mobaneg"], w=[("gsb", e_)])
                    for ti in range(8):
                        add("dve", lambda e, e_=e_, ti=ti: e.max(out=top8[:, e_ * 8 + ti, :], in_=gsb[:, e_, ti, :]),
                            r=[("gsb", e_)], w=[("top8", e_, ti)])
                        add("dve", lambda e, e_=e_, ti=ti: e.tensor_scalar(
                            out=mbtok[:, e_, ti, 0:8], in0=gsb[:, e_, ti, :],
                            scalar1=top8[:, e_ * 8 + ti, 3:4], scalar2=-1.0, op0=ALU.is_ge, op1=ALU.add),
                            r=[("gsb", e_), ("top8", e_, ti)], w=[("mbtok", e_, ti)])
                    for cc in (2, 3):
                        for ti in range(4):
                            tt = (cc - 2) * 4 + ti
                            add("pe", lambda e, e_=e_, ti=ti, tt=tt: e.transpose(
                                psb[0:32, ti * 128:(ti + 1) * 128], mbtok[:, e_, tt, :], ident[:, :]),
                                r=[("mbtok", e_, tt), "ident"], w=["psb"])
                        add("act", lambda e, e_=e_, cc=cc, qb=qb: e.activation(
                            out=QB(qb[e_])[96:128, cc * 512:(cc + 1) * 512], in_=psb[0:32, 0:512], func=AF.Copy),
                            r=["psb", "qkinit"], w=[(("QB", qb[e_]), "m", cc)])
                if KSTOP == "gate":
                    return
                for e_ in range(2):
                    h = 2 * p + e_
                    for c in range(NCK):
                        ob = 4 + nxt("ob", 2)
                        attn_chunk(c, QB(qb[e_]), ("QB", qb[e_]), KB(qb[e_]), ("KB", qb[e_]), 128,
                                   lambda j, va=va, e_=e_: VA(va)[:, j, e_, :], ("VA", va),
                                   lambda i: range(0, i + 1),
                                   lambda j, i: 0 if j == i else None, ob, True)
                        kb_ = nxt("rs", 4)
                        rinv_of(ob, 65, 64, kb_)
                        for q in range(4):
                            t = 4 * c + q
                            add("dve", lambda e, ob=ob, q=q, t=t, h=h, kb_=kb_: e.tensor_scalar(
                                out=oall(t, h * 64, h * 64 + 64), in0=ps[ob][:, q * 65:q * 65 + 64],
                                scalar1=rinv[kb_][:, q:q + 1], scalar2=None, op0=ALU.mult),
                                r=[PS(ob), ("rinv", kb_)], w=[("oall", t, h)])

        if "nsa" in mix_parts:
            nsa(l)

        if "out" in mix_parts:
            for c in range(NCK):
                for q in range(4):
                    t = 4 * c + q
                    yb = nxt("ytok", 1)
                    ork = [("oall", t, h) for h in range(16)]
                    alias_w = ["wvn", ("w1s", 0), ("w1s", 1)] if (c == 0 and q == 0) else []
                    for hf in range(2):
                        add("act", lambda e, t=t, hf=hf: e.activation(
                            out=pt[0][:, :], in_=oall(t, hf * 512, hf * 512 + 512), func=AF.Square,
                            accum_out=ssq[:, hf:hf + 1]),
                            r=ork, w=[("pt", 0), ("ssq", hf)])
                    add("act", lambda e: e.activation(out=srt[:, :], in_=ssq[:, :], func=AF.Sqrt,
                                                      bias=epsb[:, 0:1], scale=1.0 / 512),
                        r=[("ssq", 0), ("ssq", 1), "epsb"], w=["srt"])
                    add("dve", lambda e: e.reciprocal(out=srt[:, :], in_=srt[:, :]), r=["srt"], w=["srt"])
                    for hf in range(2):
                        add("dve", lambda e, t=t, hf=hf, yb=yb: e.scalar_tensor_tensor(
                            out=ytok[yb][:, hf * 512:(hf + 1) * 512], in0=oall(t, hf * 512, hf * 512 + 512),
                            scalar=srt[:, hf:hf + 1], in1=onorm[:, hf * 512:(hf + 1) * 512],
                            op0=ALU.mult, op1=ALU.mult),
                            r=ork + ["srt", "onorm"], w=[("ytok", yb, hf)] + alias_w)
                    for k in range(8):
                        add("pe", lambda e, k=k, yb=yb: e.transpose(
                            psb[:, k * 128:(k + 1) * 128], ytok[yb][:, k * 128:(k + 1) * 128], ident[:, :]),
                            r=[("ytok", yb, k // 4), "ident"], w=["psb"])
                    add("act", lambda e, q=q: e.activation(
                        out=yT[:, 0:4, q * 128:(q + 1) * 128],
                        in_=psb[:, 0:512].rearrange("p (k t) -> p k t", t=128), func=AF.Copy),
                        r=["psb"], w=[("yT", q, 0)] + alias_w)
                    add("act", lambda e, q=q: e.activation(
                        out=yT[:, 4:8, q * 128:(q + 1) * 128],
                        in_=psb[:, 512:1024].rearrange("p (k t) -> p k t", t=128), func=AF.Copy),
                        r=["psb"], w=[("yT", q, 1)] + alias_w)
                cs = slice(c * 512, (c + 1) * 512)
                for dc in range(8):
                    s_ = nxt("wf", WF_SLOTS)
                    dma_cast(wf[s_][:], wout_d[l, dc, :, :, :], f"wf{s_}", w=[("wf", s_)])
                    pb = nxt("prj", 2)
                    for k in range(8):
                        add("pe", lambda e, pb=pb, s_=s_, k=k: e.matmul(
                            ps[pb][:, :], wf[s_][:, k, :], yT[:, k, :],
                            start=(k == 0), stop=(k == 7)),
                            r=[("wf", s_)] + [("yT", q, k // 4) for q in range(4)], w=[PS(pb)])
                    add("dve", lambda e, pb=pb, dc=dc, cs=cs: e.tensor_tensor(
                        out=xT[:, dc, cs], in0=ps[pb][:, :], in1=xT[:, dc, cs], op=ALU.add),
                        r=[PS(pb), ("xT", dc, c)], w=[("xT", dc, c)])

    def nsa(l):
        va_all = [(("VA", i), tq) for i in range(2) for tq in range(4)] + [("VAone", 0), ("VAone", 1)]
        for kv in range(2):
            s_ = load_wf(l, 12 + kv)

            def evc(tc, P, pb, kv=kv):
                cs = slice(tc * 512, (tc + 1) * 512)
                for g in range(2):
                    copy_evac("act" if g == 0 else "dve", CIN(g)[64 * kv:64 * kv + 64, cs],
                              P[64 * g:64 * g + 64, :], [PS(pb)], [("CIN", g, kv, tc)] + va_all)
            proj_fm(s_, evc)
        import os
        KSTOP = os.environ.get("KSTOP", "")
        if KSTOP == "n_cin":
            return
        dma_cast(w2s[:], cw2_d[l, :, :, :, :], "w2s", w=["w2s"])
        dma_cast(cpos[:], cpos_d[l, :, :], "cpos", w=["cpos"])
        for kv in range(2):
            cb = kv
            rows = slice(64 * kv, 64 * kv + 64)
            add("pe", lambda e, cb=cb: e.matmul(ps[cb][:, 0:512], zeros[:, 0:128], zeros[:, 0:512],
                                                start=True, stop=False),
                r=["zeros"], w=[PS(cb)])
            for piece in range(8):
                s_ = nxt("w1s", 2)
                dma_cast(w1s[s_][:], cw1_d[l, :, piece * 4:(piece + 1) * 4, :], f"w1s{s_}", w=[("w1s", s_)])
                for li in range(4):
                    lpos = piece * 4 + li
                    for cc in range(2):
                        for g in range(2):
                            col = (g * 2 + cc) * NCMP
                            add("pe", lambda e, cb=cb, rows=rows, li=li, lpos=lpos, cc=cc, g=g, col=col, s_=s_:
                                e.matmul(ps[cb][:, col:col + NCMP],
                                         w1s[s_][rows, li, cc * 128:(cc + 1) * 128],
                                         CIN(g)[rows, lpos:lpos + 16 * (NCMP - 1) + 1:16],
                                         start=False, stop=False),
                                r=[("w1s", s_)] + [("CIN", g, kv, tc) for tc in range(NCK)], w=[PS(cb)])
                        add("pe", lambda e, cb=cb, rows=rows, li=li, lpos=lpos, cc=cc, s_=s_, piece=piece:
                            e.matmul(ps[cb][:, 508 + cc:509 + cc],
                                     w1s[s_][rows, li, cc * 128:(cc + 1) * 128],
                                     cpos[rows, lpos:lpos + 1],
                                     start=False, stop=(piece == 7 and li == 3 and cc == 1)),
                            r=[("w1s", s_), "cpos"], w=[PS(cb)])
            if KSTOP == "n_cmpmm":
                return
            P = ps[cb]
            add("dve", lambda e, P=P: e.tensor_copy(posb[:, 0:2], P[:, 508:510]),
                r=[PS(cb)], w=["posb"])
            for g in range(2):
                for cc in range(2):
                    col = (g * 2 + cc) * NCMP
                    add("act", lambda e, P=P, col=col, cc=cc: e.activation(
                        out=gx[:, col:col + NCMP], in_=P[:, col:col + NCMP], func=AF.Identity,
                        bias=posb[:, cc:cc + 1]),
                        r=[PS(cb), "posb"], w=[("gx", g, cc)])
            gxk = [("gx", g, cc) for g in range(2) for cc in range(2)]
            add("dve", lambda e: e.tensor_tensor(out=gu[:, :], in0=gx[:, :], in1=gx[:, :], op=ALU.mult),
                r=gxk, w=["gu"])
            add("dve", lambda e: e.tensor_scalar(out=gu[:, :], in0=gu[:, :], scalar1=0.044715,
                                                 scalar2=1.0, op0=ALU.mult, op1=ALU.add),
                r=["gu"], w=["gu"])
            add("dve", lambda e: e.tensor_tensor(out=gu[:, :], in0=gu[:, :], in1=gx[:, :], op=ALU.mult),
                r=["gu"] + gxk, w=["gu"])
            add("act", lambda e: e.activation(out=gu[:, :], in_=gu[:, :], func=AF.Sigmoid,
                                              scale=1.5957691216057308),
                r=["gu"], w=["gu"])
            add("dve", lambda e: e.tensor_tensor(out=hid[:, :], in0=gu[:, :], in1=gx[:, :], op=ALU.mult),
                r=["gu"] + gxk, w=["hid"])
            if KSTOP == "n_gelu":
                return
            for g in range(2):
                if kv == 0:
                    for cc in range(2):
                        col = (g * 2 + cc) * NCMP
                        add("pe", lambda e, cc=cc, col=col: e.matmul(
                            ps[6][0:64, 0:NCMP], w2s[:, 0, cc, :], hid[:, col:col + NCMP],
                            start=(cc == 0), stop=(cc == 1)),
                            r=["w2s", "hid"], w=[PS(6)])
                    add("act", lambda e, g=g: e.activation(out=kcT[g][0:64, 0:NCMP], in_=ps[6][0:64, 0:NCMP],
                                                           func=AF.Copy),
                        r=[PS(6)], w=[("kcT", g, "k")])
                else:
                    for cc in range(2):
                        col = (g * 2 + cc) * NCMP
                        add("pe", lambda e, cc=cc, col=col: e.matmul(
                            ps[6][0:NCMP, 128:192], hid[:, col:col + NCMP], w2s[:, 1, cc, :],
                            start=(cc == 0), stop=(cc == 1)),
                            r=["w2s", "hid"], w=[PS(6)])
                    add("dve", lambda e, g=g: e.tensor_copy(vcaug[g][0:NCMP, 0:64], ps[6][0:NCMP, 128:192]),
                        r=[PS(6)], w=[("vcaug", g, "v")])
        if KSTOP == "n_kc":
            return
        cin_all = [("CIN", g, kv, tc) for g in range(2) for kv in range(2) for tc in range(NCK)]
        for i in range(2):
            add("pool", lambda e, i=i: e.memset(VA(i)[:, :, :, 64:65], 1.0), w=[("VAone", i)] + cin_all)
        for c in range(8):
            dma_cast(wvn[:, c, :], winvn_d[l, :, c, :], "wvn", w=["wvn"])
        for t in range(NT):
            pb = nxt("prj", 2)
            for c in range(8):
                add("pe", lambda e, pb=pb, c=c, t=t: e.matmul(
                    ps[pb][:, 0:280], hT[:, c, t * 128:(t + 1) * 128], wvn[:, c, :], start=(c == 0), stop=(c == 7)),
                    r=["wvn", ("hT", c, t // 4)], w=[PS(pb)])
            for g in range(2):
                add("dve", lambda e, pb=pb, g=g, t=t: e.tensor_copy(
                    VA(g)[:, t, :, 0:64],
                    ps[pb][:, 0:256].rearrange("p (b g c) -> p b g c", b=2, g=2, c=64)[:, :, g, :]),
                    r=[PS(pb)], w=[(("VA", g), t // 4)] + (cin_all if t == 0 else []))
            add("act", lambda e, pb=pb, t=t: e.activation(out=gates[:, t, :], in_=ps[pb][:, 256:280],
                                                          func=AF.Sigmoid),
                r=[PS(pb)], w=[("gates", t)])
        if KSTOP == "n_vn":
            return
        dma_sp(KB(0)[96:128, :], c_eslc_d[:, :], "kbE0", w=[(("KB", 0), "E")])
        for g in range(2):
            for kw in range(2):
                s_ = load_wf(l, 14 + kw)

                def evs(tc, P, pb, kw=kw, g=g):
                    cs = slice(tc * 512, (tc + 1) * 512)
                    copy_evac("act" if kw == 0 else "dve", KB(kw)[0:64, cs], P[64 * g:64 * g + 64, :],
                              [PS(pb)], [(("KB", kw), "k", tc)])
                proj_fm(s_, evs)
            for r_ in range(4):
                dma_sp(QB(r_)[64:68, :], c_qalibi_d[4 * g + r_, :, :], f"qal{r_}", r=["qkinit"],
                       w=[(("QB", r_), "al")])
            for pr in range(2):
                s_ = load_wf(l, 8 + 2 * g + pr)

                def evq(tc, P, pb, pr=pr):
                    cs = slice(tc * 512, (tc + 1) * 512)
                    copy_evac("act", QB(2 * pr)[0:64, cs], P[0:64, :], [PS(pb), "qkinit"],
                              [(("QB", 2 * pr), "q", tc)])
                    copy_evac("dve", QB(2 * pr + 1)[0:64, cs], P[64:128, :], [PS(pb), "qkinit"],
                              [(("QB", 2 * pr + 1), "q", tc)])
                proj_fm(s_, evq)
            if KSTOP == "n_q":
                return
            for c in range(NCK):
                cs = slice(c * 512, (c + 1) * 512)
                if KSTOP == "n_c2start" and c == 2:
                    return
                for r_ in range(4):
                    hh = 4 * g + r_
                    sbk = 2 + nxt("st", 2)
                    ST = ps[sbk]
                    add("pe", lambda e, ST=ST, r_=r_, g=g, cs=cs: e.matmul(
                        ST[0:NCMP, :], kcT[g][0:68, 0:NCMP], QB(r_)[0:68, cs], start=True, stop=False),
                        r=[("kcT", g, "k"), ("kcT", g, "al"), (("QB", r_), "q", c), (("QB", r_), "al")], w=[PS(sbk)])
                    add("pe", lambda e, ST=ST, cs=cs: e.matmul(
                        ST[0:NCMP, :], ident[0:NCMP, 0:NCMP], cmpmask[0:NCMP, cs], start=False, stop=True),
                        r=["ident", "cmpmask"], w=[PS(sbk)])
                    pb_ = nxt("pt", 2)
                    add("act", lambda e, ST=ST, pb_=pb_: e.activation(
                        out=pt[pb_][0:NCMP, :], in_=ST[0:NCMP, :], func=AF.Exp, scale=0.125),
                        r=[PS(sbk)], w=[("pt", pb_)])
                    if KSTOP == "n_c1":
                        return
                    ob = 4 + nxt("ob", 2)
                    for q in range(4):
                        add("pe", lambda e, ob=ob, q=q, pb_=pb_, g=g: e.matmul(
                            ps[ob][:, q * 97:(q + 1) * 97], pt[pb_][0:NCMP, q * 128:(q + 1) * 128],
                            vcaug[g][0:NCMP, :], start=True, stop=True),
                            r=[("pt", pb_), ("vcaug", g, "v"), ("vcaug", g, "c")], w=[PS(ob)])
                    if KSTOP == "n_c2":
                        return
                    kb_ = nxt("rs", 4)
                    rinv_of(ob, 97, 64, kb_)
                    add("dve", lambda e, kb_=kb_, hh=hh, c=c: e.tensor_tensor(
                        out=coef[kb_][:, :], in0=rinv[kb_][:, :], in1=gates[:, 4 * c:4 * c + 4, hh * 3 + 0],
                        op=ALU.mult),
                        r=[("rinv", kb_)] + [("gates", 4 * c + q) for q in range(4)], w=[("coef", kb_)])
                    for q in range(4):
                        add("dve", lambda e, ob=ob, q=q, r_=r_, kb_=kb_: e.tensor_scalar(
                            out=oacc[:, q, r_ * 64:(r_ + 1) * 64], in0=ps[ob][:, q * 97:q * 97 + 64],
                            scalar1=coef[kb_][:, q:q + 1], scalar2=None, op0=ALU.mult),
                            r=[PS(ob), ("coef", kb_)], w=[("oacc", q, r_)])
                        if c >= 2:
                            add("dve", lambda e, ob=ob, q=q, kb_=kb_, r_=r_: e.tensor_scalar(
                                out=imp4[r_][:, q, :], in0=ps[ob][:, q * 97 + 65:q * 97 + 97],
                                scalar1=rinv[kb_][:, q:q + 1], scalar2=None, op0=ALU.mult),
                                r=[PS(ob), ("rinv", kb_)], w=[("imp4", r_, q)])
                if KSTOP == "n_cmp" and c == 2:
                    return
                if KSTOP == "n_c3":
                    return
                if c >= 2:
                    i4k = [("imp4", r_, q) for r_ in range(4) for q in range(4)]
                    add("dve", lambda e: e.tensor_tensor(
                        out=score[:, :, :], in0=imp4[0][:, :, :], in1=imp4[1][:, :, :], op=ALU.add),
                        r=i4k, w=["score"])
                    add("dve", lambda e: e.tensor_tensor(
                        out=score2[:, :, :], in0=imp4[2][:, :, :], in1=imp4[3][:, :, :], op=ALU.add),
                        r=i4k, w=[("score2", q) for q in range(4)])
                    add("dve", lambda e: e.tensor_tensor(
                        out=imp4[0][:, :, :], in0=score[:, :, :], in1=score2[:, :, :], op=ALU.add),
                        r=["score"] + [("score2", q) for q in range(4)], w=[("imp4", 0, q) for q in range(4)])
                    add("dve", lambda e, c=c: e.tensor_tensor(
                        out=score[:, :, :], in0=imp4[0][:, :, :], in1=force[:, 4 * (c - 2):4 * (c - 2) + 4, :], op=ALU.add),
                        r=[("imp4", 0, q) for q in range(4)] + ["force"], w=["score"])
                    for q in range(4):
                        add("dve", lambda e, q=q: e.max(out=t8a[:, q, :], in_=score[:, q, :]),
                            r=["score"], w=[("t8a", q)])
                        add("dve", lambda e, q=q: e.match_replace(
                            out=score2[:, q, :], in_to_replace=t8a[:, q, :], in_values=score[:, q, :],
                            imm_value=-1e30),
                            r=["score", ("t8a", q)], w=[("score2", q)])
                        add("dve", lambda e, q=q: e.max(out=t8b[:, q, :], in_=score2[:, q, :]),
                            r=[("score2", q)], w=[("t8b", q)])
                        add("dve", lambda e, q=q: e.tensor_scalar(
                            out=mbn[:, q, :], in0=score[:, q, :], scalar1=t8b[:, q, 7:8], scalar2=-1.0,
                            op0=ALU.is_ge, op1=ALU.add),
                            r=["score", ("t8b", q)], w=[("mbn", q)])
                        add("pe", lambda e, q=q: e.transpose(
                            psb[0:32, q * 128:(q + 1) * 128], mbn[:, q, :], ident[:, :]),
                            r=[("mbn", q), "ident"], w=["psb"])
                    for r_ in range(4):
                        copy_evac("act", QB(r_)[96:128, cs], psb[0:32, 0:512],
                                  ["psb", "qkinit"], [(("QB", r_), "m", c)])
                if KSTOP == "n_topk" and c == 2:
                    return
                for br in (2, 1):
                    if KSTOP == "n_win0" and br == 1:
                        return
                    for r_ in range(4):
                        hh = 4 * g + r_
                        ob = 4 + nxt("ob", 2)
                        if br == 2:
                            attn_chunk(c, QB(r_), ("QB", r_), KB(1), ("KB", 1), 68,
                                       lambda j, g=g: VA(g)[:, j, 1, :], ("VA", g),
                                       lambda i: range(max(0, i - 4), i + 1),
                                       lambda j, i: 0 if j == i else (1 if j == i - 4 else None), ob, False)
                        else:
                            attn_chunk(c, QB(r_), ("QB", r_), KB(0), ("KB", 0), 128,
                                       lambda j, g=g: VA(g)[:, j, 0, :], ("VA", g),
                                       lambda i: range(0, i + 1),
                                       lambda j, i: 0 if j == i else None, ob, True)
                        kb_ = nxt("rs", 4)
                        rinv_of(ob, 65, 64, kb_)
                        add("dve", lambda e, kb_=kb_, hh=hh, c=c, br=br: e.tensor_tensor(
                            out=coef[kb_][:, :], in0=rinv[kb_][:, :], in1=gates[:, 4 * c:4 * c + 4, hh * 3 + br],
                            op=ALU.mult),
                            r=[("rinv", kb_)] + [("gates", 4 * c + q) for q in range(4)], w=[("coef", kb_)])
                        for q in range(4):
                            add("dve", lambda e, ob=ob, q=q, r_=r_, kb_=kb_: e.scalar_tensor_tensor(
                                out=oacc[:, q, r_ * 64:(r_ + 1) * 64], in0=ps[ob][:, q * 65:q * 65 + 64],
                                scalar=coef[kb_][:, q:q + 1], in1=oacc[:, q, r_ * 64:(r_ + 1) * 64],
                                op0=ALU.mult, op1=ALU.add),
                                r=[PS(ob), ("coef", kb_), ("oacc", q, r_)], w=[("oacc", q, r_)])
                if KSTOP == "n_slc0":
                    return
                for q in range(4):
                    t = 4 * c + q
                    add("act", lambda e, q=q, t=t, g=g: e.activation(
                        out=oall(t, 512 + g * 256, 512 + (g + 1) * 256), in_=oacc[:, q, :], func=AF.Copy),
                        r=[("oacc", q, r_) for r_ in range(4)], w=[("oall", t, 8 + 4 * g + r_) for r_ in range(4)])

    for l in range(L):
        if "ffa" in stages:
            ffn(l, 0)
            sc.barrier()
        if "mix" in stages:
            mixer(l)
            sc.barrier()
        if "ffb" in stages:
            ffn(l, 1)
            sc.barrier()
    if final:
        norm_fm(12, final_out=True)
    else:
        for c in range(8):
            dma_sp(outT_d[c * 128:(c + 1) * 128, :], xT[:, c, :], "out", r=[("xT", c, k) for k in range(NCK)])
    for name in dbg:
        if name == "oall":
            dd = nc.dram_tensor("dbg_oall", [128, NT * 1024], BF16, kind="ExternalOutput").ap()
            dma_sp(dd[:, :], arena[:, 0:NT * 1024], "out", r=[])
    sc.barrier()

    sems = {e: es.enter_context(nc.semaphore(f"sem_{e}")) for e in ENGS}
    chsem = {c: es.enter_context(nc.semaphore(f"ch_{c}")) for c in sc.chan_cnt}

    def semof(src):
        return chsem[src[1]] if isinstance(src, tuple) else sems[src]

    def emit(name, e):
        for fn, waits, inc in sc.ops[name]:
            for (src, val) in waits:
                e.wait_ge(semof(src), val)
            if fn is None:
                continue
            ins = fn(e)
            if inc is True:
                ins.then_inc(sems[name], 1)
            elif inc:
                ins.then_inc(chsem[inc[1]], 16)

    with nc.Block() as block:
        @block.tensor
        def _(e):
            emit("pe", e)

        @block.scalar
        def _(e):
            emit("act", e)

        @block.vector
        def _(e):
            emit("dve", e)

        @block.gpsimd
        def _(e):
            emit("pool", e)

        @block.sync
        def _(e):
            emit("sp", e)
    es.close()
    return nc


def _bf(a):
    return np.asarray(a, dtype=np.float32).astype(ml_dtypes.bfloat16)


def make_consts():
    t = np.arange(S)
    c = {}
    c["c_ident"] = _bf(np.eye(128))
    s_ = np.arange(128)[:, None]
    t_ = np.arange(128)[None, :]
    tri = np.zeros((128, 2, 128), np.float32)
    tri[:, 0, :] = np.where(s_ <= t_, 0.0, -BIG)
    tri[:, 1, :] = np.where(s_ > t_, 0.0, -BIG)
    c["c_tri"] = _bf(tri)
    cend = 16 * np.arange(128) + 31
    cm = np.where(cend[:, None] <= t[None, :], 0.0, -BIG)
    cm[127, :] = -BIG
    c["c_cmpmask"] = _bf(cm)
    slopes = 2.0 ** (-np.arange(1, 9))
    qa = np.zeros((8, 4, S), np.float32)
    for h in range(8):
        qa[h, 0] = 8 * slopes[h]
        qa[h, 1] = 8 * slopes[h]
        qa[h, 2] = -8 * slopes[h] * 64 * (t // 64)
        qa[h, 3] = -8 * slopes[h] * (t % 64)
    c["c_qalibi"] = _bf(qa)
    ka = np.stack([64.0 * (t // 64), 1.0 * (t % 64), np.ones(S), np.ones(S)]).astype(np.float32)
    c["c_kalibi"] = _bf(ka)
    kac = np.stack([64.0 * (cend // 64), 1.0 * (cend % 64), np.ones(128), np.ones(128)]).astype(np.float32)
    c["c_kalibic"] = _bf(kac)
    em = np.zeros((32, S), np.float32)
    for n in range(8):
        em[n, n * 256:(n + 1) * 256] = BIG
    c["c_emoba"] = _bf(em)
    esl = np.zeros((32, S), np.float32)
    for j in range(32):
        esl[j, j * 64:(j + 1) * 64] = BIG
    c["c_eslc"] = _bf(esl)
    force = np.zeros((128, 8, 32), np.float32)
    for ti in range(8):
        tt = (8 + ti) * 128 + np.arange(128)
        tb = tt // 64
        jj = np.arange(32)[None, :]
        cand = jj <= tb[:, None]
        forced = (jj == 0) | (jj == tb[:, None]) | (jj == tb[:, None] - 1)
        force[:, ti, :] = np.where(cand, np.where(forced, 1e4, 0.0), -1e30)
    c["c_force"] = force
    mn = np.zeros((128, 8, 8), np.float32)
    for ti in range(8):
        own = (8 + ti) // 2
        for n in range(8):
            mn[:, ti, n] = 0.0 if n < own else (1e30 if n == own else -1e30)
    c["c_mobaneg"] = mn
    va = np.zeros((128, 33), np.float32)
    va[:, 0] = 1.0
    for n in range(NCMP):
        for j in range(32):
            if (16 * n < 64 * j + 64) and (16 * n + 32 > 64 * j):
                va[n, 1 + j] = 1.0
    c["c_vcaug"] = _bf(va)
    return c


def prep_weights(inp):
    f = lambda a: np.ascontiguousarray(np.asarray(a, dtype=np.float32))
    L = L_FULL
    w = {}
    g = np.zeros((128, 13, 8), np.float32)
    for l in range(L):
        for k, nm in enumerate(("ffa_norm", "mix_norm", "ffb_norm")):
            g[:, 3 * l + k, :] = np.asarray(inp[nm])[l].reshape(8, 128).T
    g[:, 12, :] = np.asarray(inp["final_norm"]).reshape(8, 128).T
    w["gains"] = g
    wgu = np.empty((L, 2, NF, 128, 2, 8, 128), np.float32)
    wd = np.empty((L, 2, 2, 8, 128, 11, 128), np.float32)
    ffw = ((inp["ffa_w_gate"], inp["ffa_w_up"], inp["ffa_w_down"]),
           (inp["ffb_w_gate"], inp["ffb_w_up"], inp["ffb_w_down"]))
    for ab in range(2):
        for gi in range(2):
            a = np.asarray(ffw[ab][gi]).reshape(L, 8, 128, NF, 128)
            wgu[:, ab, :, :, gi, :, :] = a.transpose(0, 3, 2, 1, 4)
        a = np.asarray(ffw[ab][2]).reshape(L, 2, 11, 128, 8, 128)
        wd[:, ab] = a.transpose(0, 1, 4, 3, 2, 5)
    w["wgu"] = wgu
    w["wd"] = wd
    win = np.asarray(inp["w_in"]).reshape(L, 8, 128, 2840)
    cols = [0, 128, 256, 384, 512, 640, 768, 896, 1536, 1664, 1792, 1920, 2048, 2176, 2304, 2560]
    w["winf"] = f(np.stack([win[:, :, :, c0:c0 + 128].transpose(0, 2, 1, 3) for c0 in cols], axis=1))
    w["winvm"] = f(np.stack([win[:, :, :, 1024 + 128 * p:1024 + 128 * (p + 1)].transpose(0, 2, 1, 3)
                             for p in range(4)], axis=1))
    vn = np.concatenate([win[:, :, :, 2432:2560], win[:, :, :, 2688:2816], win[:, :, :, 2816:2840]], axis=-1)
    w["winvn"] = f(vn.transpose(0, 2, 1, 3))
    w["wout"] = f(np.asarray(inp["w_out"]).reshape(L, 8, 128, 8, 128).transpose(0, 3, 2, 1, 4))
    w1 = np.stack([np.asarray(inp["cmp_k_w1"]), np.asarray(inp["cmp_v_w1"])], axis=1)
    w1 = w1.reshape(L, 2, 32, 64, 256).transpose(0, 1, 3, 2, 4).reshape(L, 128, 32, 256)
    w["cw1"] = f(w1)
    w2 = np.stack([np.asarray(inp["cmp_k_w2"]), np.asarray(inp["cmp_v_w2"])], axis=1)
    w["cw2"] = f(w2.reshape(L, 2, 2, 128, 64).transpose(0, 3, 1, 2, 4))
    pos = np.stack([np.asarray(inp["cmp_pos_k"]), np.asarray(inp["cmp_pos_v"])], axis=1)
    w["cpos"] = f(pos.transpose(0, 1, 3, 2).reshape(L, 128, 32))
    on = np.concatenate([np.asarray(inp["moba_out_norm"]), np.asarray(inp["nsa_out_norm"])], axis=-1)
    w["onorm"] = f(np.broadcast_to(on[:, None, :], (L, 128, 1024)))
    return w


_NC_CACHE = {}


def kernel(**inputs):
    x = np.asarray(inputs["x"], dtype=np.float32)
    B = x.shape[0]
    shared = {}
    shared.update(make_consts())
    shared.update(prep_weights(inputs))
    if "full" not in _NC_CACHE:
        _NC_CACHE["full"] = build()
    nc = _NC_CACHE["full"]
    in_maps = []
    for b in range(B):
        m = dict(shared)
        m["xT"] = np.ascontiguousarray(x[b].T)
        in_maps.append(m)
    res = run_bass_kernel_spmd(nc, in_maps, core_ids=list(range(B)))
    out = np.stack([np.ascontiguousarray(r["outT"].T) for r in res.results], axis=0)
    return out.astype(np.float32)
```

```python
import bisect
from contextlib import ExitStack

import numpy as np
import ml_dtypes

import concourse.bass as bass
import concourse.mybir as mybir
from concourse.bass_utils import run_bass_kernel_spmd

F32 = mybir.dt.float32
BF16 = mybir.dt.bfloat16
AF = mybir.ActivationFunctionType
ALU = mybir.AluOpType
AX = mybir.AxisListType

S = 2048
D = 1024
DFF = 2816
NF = 22
NT = 16
NCK = 4
L_FULL = 4
EPS = 1e-6
BIG = 32768.0
NCMP = 127

ENGS = ("pe", "act", "dve", "pool", "sp")


class Sched:
    def __init__(self):
        self.ops = {e: [] for e in ENGS}
        self.known = {e: {} for e in ENGS}
        self.hist = {e: [(-1, {})] for e in ENGS}
        self.hist_idx = {e: [-1] for e in ENGS}
        self.marks = {e: [] for e in ENGS}
        self.lastw = {}
        self.readers = {}
        self.chan_cnt = {}
        self.last_compute = {e: -1 for e in ENGS}

    def _snapshot(self, eng):
        idx = len(self.ops[eng])
        self.hist[eng].append((idx, dict(self.known[eng])))
        self.hist_idx[eng].append(idx)

    def _clock(self, eng, idx):
        pos = bisect.bisect_right(self.hist_idx[eng], idx) - 1
        return self.hist[eng][pos][1]

    def _need(self, eng, src, idx, waits):
        kn = self.known[eng]
        if isinstance(src, tuple):
            if kn.get(src, 0) >= idx:
                return False
            tot = self.chan_cnt[src[1]]
            waits.append((src, tot * 16))
            kn[src] = tot
            return True
        if kn.get(src, -1) >= idx:
            return False
        mk = self.marks[src]
        pos = bisect.bisect_left(mk, idx)
        if pos == len(mk):
            mk.append(idx)
            self.ops[src][idx][2] = True
        midx = mk[pos]
        waits.append((src, pos + 1))
        kn[src] = midx
        for s2, v2 in self._clock(src, midx).items():
            if isinstance(s2, tuple):
                if kn.get(s2, 0) < v2:
                    kn[s2] = v2
            elif kn.get(s2, -1) < v2:
                kn[s2] = v2
        return True

    def add(self, eng, fn, r=(), w=(), ch=None):
        deps = []
        lastw = self.lastw
        readers = self.readers
        for b in r:
            d = lastw.get(b)
            if d is not None:
                deps.append(d)
        skip_same = (eng == "pe")
        for b in w:
            d = lastw.get(b)
            if d is not None and not (skip_same and d[0] == eng):
                deps.append(d)
            for d in readers.get(b, ()):
                if not (skip_same and d[0] == eng):
                    deps.append(d)
        waits = []
        changed = False
        for (src, idx) in deps:
            if self._need(eng, src, idx, waits):
                changed = True
        idx = len(self.ops[eng])
        if changed:
            self._snapshot(eng)
        self.ops[eng].append([fn, waits, False])
        if ch is not None:
            self.chan_cnt[ch] = self.chan_cnt.get(ch, 0) + 1
            me = (("ch", ch), self.chan_cnt[ch])
            self.ops[eng][idx][2] = ("ch", ch)
        else:
            me = (eng, idx)
            self.last_compute[eng] = idx
        for b in r:
            readers.setdefault(b, []).append(me)
        for b in w:
            lastw[b] = me
            readers[b] = []
        return idx

    def barrier(self):
        for e in ENGS:
            waits = []
            ch = False
            for f in ENGS:
                if f != e and self.last_compute[f] >= 0:
                    ch |= self._need(e, f, self.last_compute[f], waits)
            for c, n in self.chan_cnt.items():
                ch |= self._need(e, ("ch", c), n, waits)
            if ch:
                self._snapshot(e)
            self.ops[e].append([None, waits, False])
        self.lastw = {}
        self.readers = {}


def build(L=L_FULL, final=True, stages=("ffa", "mix", "ffb"), dbg=(), mix_parts=("moba", "nsa", "out")):
    nc = bass.Bass("TRN2", target_bir_lowering=False)
    sc = Sched()
    es = ExitStack()

    def din(name, shape, dt=F32):
        return nc.dram_tensor(name, list(shape), dt, kind="ExternalInput").ap()

    xT_d = din("xT", [D, S])
    gains_d = din("gains", [128, 13, 8])
    wgu_d = din("wgu", [L_FULL, 2, NF, 128, 2, 8, 128])
    wd_d = din("wd", [L_FULL, 2, 2, 8, 128, 11, 128])
    winf_d = din("winf", [L_FULL, 16, 128, 8, 128])
    winvm_d = din("winvm", [L_FULL, 4, 128, 8, 128])
    winvn_d = din("winvn", [L_FULL, 128, 8, 280])
    wout_d = din("wout", [L_FULL, 8, 128, 8, 128])
    cw1_d = din("cw1", [L_FULL, 128, 32, 256])
    cw2_d = din("cw2", [L_FULL, 128, 2, 2, 64])
    cpos_d = din("cpos", [L_FULL, 128, 32])
    onorm_d = din("onorm", [L_FULL, 128, 1024])
    c_ident_d = din("c_ident", [128, 128], BF16)
    c_tri_d = din("c_tri", [128, 2, 128], BF16)
    c_cmpmask_d = din("c_cmpmask", [128, S], BF16)
    c_qalibi_d = din("c_qalibi", [8, 4, S], BF16)
    c_kalibi_d = din("c_kalibi", [4, S], BF16)
    c_kalibic_d = din("c_kalibic", [4, 128], BF16)
    c_emoba_d = din("c_emoba", [32, S], BF16)
    c_eslc_d = din("c_eslc", [32, S], BF16)
    c_force_d = din("c_force", [128, 8, 32])
    c_mobaneg_d = din("c_mobaneg", [128, 8, 8])
    c_vcaug_d = din("c_vcaug", [128, 33], BF16)
    outT_d = nc.dram_tensor("outT", [D, S], F32, kind="ExternalOutput").ap()
    dbg_d = {}

    DTB = {F32: 4, BF16: 2}
    SB_BASE = 16544
    alloc_state = {"off": SB_BASE, "max": 0}

    def sb(name, shape, dt):
        n = 1
        for s_ in shape[1:]:
            n *= s_
        nbytes = (n * DTB[dt] + 31) // 32 * 32
        off = alloc_state["off"]
        t = nc.alloc_sbuf_tensor_at(name, list(shape), dt, offset=off)
        alloc_state["off"] = off + nbytes
        alloc_state["max"] = max(alloc_state["max"], off + nbytes)
        return t

    xT = sb("xT", [128, 8, S], F32)
    hT = sb("hT", [128, 8, S], BF16)
    NA_OALL = NT * 1024
    NA_VA = 2 * NT * 130
    OFF_VA = NA_OALL
    OFF_QB = OFF_VA + NA_VA
    OFF_KB = OFF_QB + 4 * S
    NA = OFF_KB + 2 * S
    assert NA >= 11 * S
    arena = sb("arena", [128, NA], BF16)
    gains = sb("gains_sb", [128, 13, 8], F32)
    ident = sb("ident", [128, 128], BF16)
    tri = sb("tri", [128, 2, 128], BF16)
    cmpmask = sb("cmpmask", [128, S], BF16)
    ones = sb("ones", [128, 128], BF16)
    zeros = sb("zeros", [128, 512], BF16)
    force = sb("force", [128, 8, 32], F32)
    mobaneg = sb("mobaneg", [128, 8, 8], F32)
    epsb = sb("epsb", [128, 1], F32)
    kcT = [sb(f"kcT{g}", [128, 128], BF16) for g in range(2)]
    vcaug = [sb(f"vcaug{g}", [128, 97], BF16) for g in range(2)]
    ksumf = sb("ksumf", [64, 2, 8], F32)
    ksumb = sb("ksumb", [64, 2, 8], BF16)
    gsb = sb("gsb", [128, 2, 8, 8], F32)
    top8 = sb("top8", [128, 16, 8], F32)
    mbtok = sb("mbtok", [128, 2, 8, 32], BF16)
    rs = [sb(f"rs{i}", [128, 4], F32) for i in range(4)]
    rinv = [sb(f"rinv{i}", [128, 4], F32) for i in range(4)]
    coef = [sb(f"coef{i}", [128, 4], F32) for i in range(4)]
    gates = sb("gates", [128, NT, 24], F32)
    imp = sb("imp", [128, 4, 32], F32)
    impraw = sb("impraw", [128, 4, 32], F32)
    imptmp = sb("imptmp", [128, 4, 32], F32)
    imp3 = sb("imp3", [128, 4, 32], F32)
    imp4 = [imp, impraw, imptmp, imp3]
    score = sb("score", [128, 4, 32], F32)
    score2 = sb("score2", [128, 4, 32], F32)
    t8a = sb("t8a", [128, 4, 8], F32)
    t8b = sb("t8b", [128, 4, 8], F32)
    mbn = sb("mbn", [128, 4, 32], BF16)
    posb = sb("posb", [128, 2], F32)
    ssq = sb("ssq", [128, 2], F32)
    srt = sb("srt", [128, 2], F32)
    off_n = alloc_state["off"]
    sq = [sb("sq0", [128, 8, 256], BF16)]
    rstd = [sb(f"rstd{i}", [128, 256], F32) for i in range(2)]
    end_n = alloc_state["off"]
    alloc_state["off"] = off_n
    gx = sb("gx", [128, 508], F32)
    gu = sb("gu", [128, 508], F32)
    hid = sb("hid", [128, 508], BF16)
    assert alloc_state["off"] <= end_n, (alloc_state["off"], end_n)
    alloc_state["off"] = end_n
    off_u = alloc_state["off"]
    WGU_SLOTS = 2
    wgu = [sb(f"wgu{i}", [128, 2, 8, 128], BF16) for i in range(WGU_SLOTS)]
    WD_SLOTS = 2
    wdw = [sb(f"wdw{i}", [128, 11, 128], BF16) for i in range(WD_SLOTS)]
    sg = [sb(f"sg{i}", [128, 512], BF16) for i in range(2)]
    outbuf = [sb(f"outbuf{i}", [128, 256], F32) for i in range(2)]
    alloc_state["off"] = off_u
    WF_SLOTS = 3
    wf = [sb(f"wf{i}", [128, 8, 128], BF16) for i in range(WF_SLOTS)]
    onorm = sb("onorm_sb", [128, 1024], F32)
    w2s = sb("w2s", [128, 2, 2, 64], BF16)
    cpos = sb("cpos_sb", [128, 32], BF16)
    pt = [sb(f"pt{i}", [128, 512], BF16) for i in range(2)]
    oacc = sb("oacc", [128, 4, 256], F32)
    off_a = alloc_state["off"]
    w1s = [sb(f"w1s{i}", [128, 4, 256], BF16) for i in range(2)]
    _save = alloc_state["off"]
    alloc_state["off"] = off_a + 2048
    pt = pt + [sb(f"pt{i}", [128, 512], BF16) for i in (2, 3)]
    alloc_state["off"] = _save
    wvn = sb("wvn", [128, 8, 280], BF16)
    end_a = alloc_state["off"]
    alloc_state["off"] = off_a
    ytok = [sb(f"ytok{i}", [128, 1024], BF16) for i in range(1)]
    alloc_state["off"] = max(alloc_state["off"], end_a)
    assert alloc_state["max"] <= 229376, alloc_state["max"]

    ps = [es.enter_context(nc.psum_tensor(f"ps{i}", [128, 512], F32)) for i in range(7)]
    psb = es.enter_context(nc.psum_tensor("psb", [128, 1024], BF16))

    def PS(i):
        return ("ps", i)

    def oall(tile, a, b):
        return arena[:, tile * 1024 + a: tile * 1024 + b]

    def QB(i):
        return arena[:, OFF_QB + i * S: OFF_QB + (i + 1) * S]

    def KB(i):
        return arena[:, OFF_KB + i * S: OFF_KB + (i + 1) * S]

    def VA(i):
        return arena[:, OFF_VA + i * NT * 130: OFF_VA + (i + 1) * NT * 130].rearrange(
            "p (t h c) -> p t h c", h=2, c=65)

    def CIN(g):
        return arena[:, OFF_VA + g * S: OFF_VA + (g + 1) * S]

    def AT(fi):
        return arena[:, fi * S:(fi + 1) * S]

    add = sc.add
    rot = {}

    def nxt(name, n):
        v = rot.get(name, 0)
        rot[name] = v + 1
        return v % n

    def dma_sp(out, in_, ch, r=(), w=()):
        add("sp", lambda e, o=out, i=in_: e.dma_start(out=o, in_=i), r=r, w=w, ch=ch)

    def dma_cast(out, in_, ch, r=(), w=()):
        add("pool", lambda e, o=out, i=in_: e.dma_start(out=o, in_=i), r=r, w=w, ch=ch)

    for c in range(8):
        dma_sp(xT[:, c, :], xT_d[c * 128:(c + 1) * 128, :], "x", w=[("xT", c, k) for k in range(NCK)])
    dma_sp(gains[:], gains_d[:, :, :], "c0", w=["gains"])
    dma_sp(ident[:], c_ident_d[:, :], "c0", w=["ident"])
    dma_sp(tri[:], c_tri_d[:, :, :], "c0", w=["tri"])
    dma_sp(cmpmask[:], c_cmpmask_d[:, :], "c0", w=["cmpmask"])
    dma_sp(force[:], c_force_d[:, :, :], "c0", w=["force"])
    dma_sp(mobaneg[:], c_mobaneg_d[:, :, :], "c0", w=["mobaneg"])
    for g in range(2):
        dma_sp(kcT[g][64:68, :], c_kalibic_d[:, :], "c0", w=[("kcT", g, "al")])
        dma_sp(vcaug[g][:, 64:97], c_vcaug_d[:, :], "c0", w=[("vcaug", g, "c")])
    add("dve", lambda e: e.memset(ones[:], 1.0), w=["ones"])
    add("dve", lambda e: e.memset(zeros[:], 0.0), w=["zeros"])
    add("dve", lambda e: e.memset(epsb[:], EPS), w=["epsb"])
    add("dve", lambda e: e.memset(mbtok[:], 0.0), w=["mbtok"])

    if dbg:
        add("pool", lambda e: e.memset(arena[:, 0:NA_OALL], 0.0),
            w=[("oall", t, h) for t in range(NT) for h in range(16)])

    def norm_fm(gidx, final_out=False):
        for hc in range(8):
            tc = hc // 2
            cs = slice(hc * 256, (hc + 1) * 256)
            xk = [("xT", c, tc) for c in range(8)]
            add("act", lambda e, cs=cs: e.activation(out=sq[0][:], in_=xT[:, :, cs], func=AF.Square),
                r=xk, w=[("sq", 0)])
            for c in range(8):
                add("pe", lambda e, c=c: e.matmul(ps[6][:, 0:256], ones[:, :], sq[0][:, c, :],
                                                  start=(c == 0), stop=(c == 7)),
                    r=[("sq", 0), "ones"], w=[PS(6)])
            b = nxt("rstd", 2)
            add("act", lambda e, b=b: e.activation(out=rstd[b][:], in_=ps[6][:, 0:256], func=AF.Sqrt,
                                                   bias=epsb[:, 0:1], scale=1.0 / D),
                r=[PS(6), "epsb"], w=[("rstd", b)])
            add("dve", lambda e, b=b: e.reciprocal(out=rstd[b][:], in_=rstd[b][:]),
                r=[("rstd", b)], w=[("rstd", b)])
            for c in range(8):
                if not final_out:
                    add("dve", lambda e, b=b, c=c, cs=cs: e.scalar_tensor_tensor(
                        out=hT[:, c, cs], in0=xT[:, c, cs], scalar=gains[:, gidx, c:c + 1], in1=rstd[b][:],
                        op0=ALU.mult, op1=ALU.mult),
                        r=[("xT", c, tc), ("rstd", b), "gains"], w=[("hT", c, tc)])
                else:
                    ob = nxt("outbuf", 2)
                    add("dve", lambda e, b=b, c=c, cs=cs, ob=ob: e.scalar_tensor_tensor(
                        out=outbuf[ob][:], in0=xT[:, c, cs], scalar=gains[:, gidx, c:c + 1], in1=rstd[b][:],
                        op0=ALU.mult, op1=ALU.mult),
                        r=[("xT", c, tc), ("rstd", b), "gains"], w=[("outbuf", ob)])
                    dma_sp(outT_d[c * 128:(c + 1) * 128, cs], outbuf[ob][:], "out", r=[("outbuf", ob)])

    def ffn(l, ab):
        norm_fm(3 * l + (0 if ab == 0 else 2))
        for half in range(2):
            for fi in range(11):
                f = half * 11 + fi
                s_ = nxt("wgu", WGU_SLOTS)
                for gu_ in range(2):
                    dma_cast(wgu[s_][:, gu_, :, :], wgu_d[l, ab, f, :, gu_, :, :], f"wgu{s_}", w=[("wgu", s_)])
                for tc in range(NCK):
                    cs = slice(tc * 512, (tc + 1) * 512)
                    pb = nxt("ffn_ps", 2)
                    G, U = ps[pb], ps[2 + pb]
                    for gu_, P in ((0, G), (1, U)):
                        for c in range(8):
                            add("pe", lambda e, P=P, s_=s_, gu_=gu_, c=c, cs=cs: e.matmul(
                                P[:, :], wgu[s_][:, gu_, c, :], hT[:, c, cs], start=(c == 0), stop=(c == 7)),
                                r=[("wgu", s_), ("hT", c, tc)], w=[PS(pb + 2 * gu_)])
                    sb_ = nxt("sg", 2)
                    add("act", lambda e, G=G, sb_=sb_: e.activation(out=sg[sb_][:], in_=G[:, :], func=AF.Silu),
                        r=[PS(pb)], w=[("sg", sb_)])
                    add("dve", lambda e, U=U, sb_=sb_, fi=fi, cs=cs: e.tensor_tensor(
                        out=AT(fi)[:, cs], in0=U[:, :], in1=sg[sb_][:], op=ALU.mult),
                        r=[PS(2 + pb), ("sg", sb_)], w=[("AT", fi, tc)])
            for dc in range(8):
                s_ = nxt("wd", WD_SLOTS)
                dma_cast(wdw[s_][:], wd_d[l, ab, half, dc, :, :, :], f"wd{s_}", w=[("wd", s_)])
                for tc in range(NCK):
                    cs = slice(tc * 512, (tc + 1) * 512)
                    pb = 4 + nxt("ffn_pd", 2)
                    for k in range(11):
                        add("pe", lambda e, pb=pb, s_=s_, k=k, cs=cs: e.matmul(
                            ps[pb][:, :], wdw[s_][:, k, :], AT(k)[:, cs], start=(k == 0), stop=(k == 10)),
                            r=[("wd", s_), ("AT", k, tc)], w=[PS(pb)])
                    add("dve", lambda e, pb=pb, dc=dc, cs=cs: e.scalar_tensor_tensor(
                        out=xT[:, dc, cs], in0=ps[pb][:, :], scalar=0.5, in1=xT[:, dc, cs],
                        op0=ALU.mult, op1=ALU.add),
                        r=[PS(pb), ("xT", dc, tc)], w=[("xT", dc, tc)])

    def load_wf(l, blk):
        s_ = nxt("wf", WF_SLOTS)
        dma_cast(wf[s_][:], winf_d[l, blk, :, :, :], f"wf{s_}", w=[("wf", s_)])
        return s_

    def proj_fm(s_, evac):
        for tc in range(NCK):
            cs = slice(tc * 512, (tc + 1) * 512)
            pb = nxt("prj", 2)
            for c in range(8):
                add("pe", lambda e, pb=pb, s_=s_, c=c, cs=cs: e.matmul(
                    ps[pb][:, :], wf[s_][:, c, :], hT[:, c, cs], start=(c == 0), stop=(c == 7)),
                    r=[("wf", s_), ("hT", c, tc)], w=[PS(pb)])
            evac(tc, ps[pb], pb)

    def copy_evac(eng, out, in_, r, w):
        if eng == "act":
            add("act", lambda e: e.activation(out=out, in_=in_, func=AF.Copy), r=r, w=w)
        else:
            add(eng, lambda e: e.tensor_copy(out, in_), r=r, w=w)

    def attn_chunk(c, Qap, qkey, Kap, kkey, krows, vfn, vkey, kts, special, obank, mask_in_q):
        tiles = list(range(4 * c, 4 * c + 4))
        O = ps[obank]
        add("pe", lambda e: e.matmul(O[:, 0:260], zeros[:, 0:128], zeros[:, 0:260], start=True, stop=False),
            r=["zeros"], w=[PS(obank)])
        alljs = sorted(set(j for i in tiles for j in kts(i)))
        lastj = {i: max(kts(i)) for i in tiles}
        qreads = [(qkey, "q", c), (qkey, "al"), "qkinit"]
        if mask_in_q:
            qreads.append((qkey, "m", c))
        def emit_pv(pb_, iset, i_lo, j):
            for i in iset:
                q = i - 4 * c
                add("pe", lambda e, pb_=pb_, i=i, i_lo=i_lo, q=q, j=j, last=(j == lastj[i] and i == tiles[-1]):
                    e.matmul(O[:, q * 65:(q + 1) * 65], pt[pb_][:, (i - i_lo) * 128:(i - i_lo + 1) * 128],
                             vfn(j), start=False, stop=last),
                    r=[("pt", pb_), (vkey, j // 4)], w=[PS(obank)])

        pend = []
        for j in alljs:
            iset = [i for i in tiles if j in kts(i)]
            i_lo, i_hi = min(iset), max(iset)
            N = (i_hi - i_lo + 1) * 128
            sbk = nxt("st", 4)
            ST = ps[sbk]
            specs = [(i, special(j, i)) for i in iset if special(j, i) is not None]
            add("pe", lambda e, ST=ST, j=j, i_lo=i_lo, i_hi=i_hi, N=N, ns=len(specs): e.matmul(
                ST[:, 0:N], Kap[0:krows, j * 128:(j + 1) * 128], Qap[0:krows, i_lo * 128:(i_hi + 1) * 128],
                start=True, stop=(ns == 0)),
                r=qreads + [(kkey, "k", j // 4), (kkey, "al"), (kkey, "E")], w=[PS(sbk)])
            for si, (i, kind) in enumerate(specs):
                add("pe", lambda e, ST=ST, i=i, i_lo=i_lo, kind=kind, last=(si == len(specs) - 1): e.matmul(
                    ST[:, (i - i_lo) * 128:(i - i_lo + 1) * 128], ident[:, :], tri[:, kind, :],
                    start=False, stop=last),
                    r=["ident", "tri"], w=[PS(sbk)])
            pb_ = nxt("pt", 4)
            add("act", lambda e, ST=ST, N=N, pb_=pb_: e.activation(
                out=pt[pb_][:, 0:N], in_=ST[:, 0:N], func=AF.Exp, scale=0.125),
                r=[PS(sbk)], w=[("pt", pb_)])
            pend.append((pb_, iset, i_lo, j))
            if len(pend) > 2:
                emit_pv(*pend.pop(0))
        while pend:
            emit_pv(*pend.pop(0))

    def rinv_of(obank, stride, col, kbuf):
        O = ps[obank]
        add("dve", lambda e: e.tensor_scalar(
            out=rs[kbuf][:, :], in0=O[:, 0:4 * stride].rearrange("p (q c) -> p q c", c=stride)[:, :, col],
            scalar1=1e-30, scalar2=None, op0=ALU.max),
            r=[PS(obank)], w=[("rs", kbuf)])
        add("dve", lambda e: e.reciprocal(out=rinv[kbuf][:, :], in_=rs[kbuf][:, :]),
            r=[("rs", kbuf)], w=[("rinv", kbuf)])

    def mixer(l):
        norm_fm(3 * l + 1)
        dma_sp(onorm[:], onorm_d[l, :, :], "onorm", w=["onorm"])
        add("pool", lambda e: e.memset(arena[:, OFF_QB:NA], 0.0), w=["qkinit"])
        for i in range(2):
            dma_sp(KB(i)[64:68, :], c_kalibi_d[:, :], f"kbal{i}", r=["qkinit"], w=[(("KB", i), "al")])
        for i in range(2):
            dma_sp(KB(i)[96:128, :], c_emoba_d[:, :], f"kbE{i}", r=["qkinit"], w=[(("KB", i), "E")])
        for i in range(2):
            add("pool", lambda e, i=i: e.memset(VA(i)[:, :, :, 64:65], 1.0), w=[("VAone", i)])

        import os
        KSTOP = os.environ.get("KSTOP", "")
        if KSTOP == "init":
            return
        if "moba" in mix_parts:
            for p in range(4 if not KSTOP else 1):
                qb = [0, 1]
                va = p % 2
                for e_ in range(2):
                    h = 2 * p + e_
                    dma_sp(QB(qb[e_])[64:68, :], c_qalibi_d[h, :, :], f"qal{qb[e_]}", r=["qkinit"],
                           w=[(("QB", qb[e_]), "al")])
                s_ = load_wf(l, p)

                def evq(tc, P, pb, qb=qb):
                    cs = slice(tc * 512, (tc + 1) * 512)
                    copy_evac("act", QB(qb[0])[0:64, cs], P[0:64, :], [PS(pb), "qkinit"], [(("QB", qb[0]), "q", tc)])
                    copy_evac("dve", QB(qb[1])[0:64, cs], P[64:128, :], [PS(pb), "qkinit"], [(("QB", qb[1]), "q", tc)])
                proj_fm(s_, evq)
                s_ = load_wf(l, 4 + p)

                def evk(tc, P, pb, qb=qb):
                    for e_ in range(2):
                        for hf in range(2):
                            c0 = tc * 512 + hf * 256
                            add("act", lambda e, e_=e_, hf=hf, c0=c0, P=P, tc=tc: e.activation(
                                out=KB(qb[e_])[0:64, c0:c0 + 256], in_=P[64 * e_:64 * e_ + 64, hf * 256:hf * 256 + 256],
                                func=AF.Copy, accum_out=ksumf[0:64, e_, 2 * tc + hf:2 * tc + hf + 1]),
                                r=[PS(pb), "qkinit"], w=[(("KB", qb[e_]), "k", tc), ("ksumf", e_, tc)])
                proj_fm(s_, evk)
                add("dve", lambda e: e.tensor_copy(ksumb[:, :, :], ksumf[:, :, :]),
                    r=[("ksumf", e_, tc) for e_ in range(2) for tc in range(NCK)], w=["ksumb"])
                if KSTOP == "qk":
                    return
                s_ = nxt("wf", WF_SLOTS)
                dma_cast(wf[s_][:], winvm_d[l, p, :, :, :], f"wf{s_}", w=[("wf", s_)])
                for tq in range(4):
                    pb = nxt("prj", 2)
                    for ti in range(4):
                        t = tq * 4 + ti
                        for c in range(8):
                            add("pe", lambda e, pb=pb, s_=s_, c=c, t=t, ti=ti: e.matmul(
                                ps[pb][:, ti * 128:(ti + 1) * 128], hT[:, c, t * 128:(t + 1) * 128], wf[s_][:, c, :],
                                start=(c == 0), stop=(c == 7)),
                                r=[("wf", s_), ("hT", c, t // 4)], w=[PS(pb)])
                    add("dve", lambda e, pb=pb, tq=tq, va=va: e.tensor_copy(
                        VA(va)[:, 4 * tq:4 * tq + 4, :, 0:64],
                        ps[pb][:, :].rearrange("p (t h c) -> p t h c", h=2, c=64)),
                        r=[PS(pb)], w=[(("VA", va), tq)])
                if KSTOP == "v":
                    return
                for e_ in range(2):
                    for ti in range(8):
                        t = 8 + ti
                        add("pe", lambda e, e_=e_, ti=ti, t=t, qb=qb: e.matmul(
                            ps[6][:, (e_ * 8 + ti) * 8:(e_ * 8 + ti) * 8 + 8],
                            QB(qb[e_])[0:64, t * 128:(t + 1) * 128], ksumb[0:64, e_, :], start=True, stop=True),
                            r=[(("QB", qb[e_]), "q", t // 4), "ksumb"], w=[PS(6)])
                for e_ in range(2):
                    add("dve", lambda e, e_=e_: e.tensor_tensor(
                        out=gsb[:, e_, :, :], in0=ps[6][:, e_ * 64:(e_ + 1) * 64].rearrange("p (t n) -> p t n", n=8),
                        in1=mobaneg[:, :, :], op=ALU.add),
                        r=[PS(6), "mobaneg"], w=[("gsb", e_)])
                    for ti in range(8):
                        add("dve", lambda e, e_=e_, ti=ti: e.max(out=top8[:, e_ * 8 + ti, :], in_=gsb[:, e_, ti, :]),
                            r=[("gsb", e_)], w=[("top8", e_, ti)])
                        add("dve", lambda e, e_=e_, ti=ti: e.tensor_scalar(
                            out=mbtok[:, e_, ti, 0:8], in0=gsb[:, e_, ti, :],
                            scalar1=top8[:, e_ * 8 + ti, 3:4], scalar2=-1.0, op0=ALU.is_ge, op1=ALU.add),
                            r=[("gsb", e_), ("top8", e_, ti)], w=[("mbtok", e_, ti)])
                if KSTOP == "gate":
                    return

                def moba_attn(e_, c):
                    h = 2 * p + e_
                    ob = 4 + nxt("ob", 2)
                    attn_chunk(c, QB(qb[e_]), ("QB", qb[e_]), KB(qb[e_]), ("KB", qb[e_]), 128,
                               lambda j, va=va, e_=e_: VA(va)[:, j, e_, :], ("VA", va),
                               lambda i: range(0, i + 1),
                               lambda j, i: 0 if j == i else None, ob, True)
                    kb_ = nxt("rs", 4)
                    rinv_of(ob, 65, 64, kb_)
                    for q in range(4):
                        t = 4 * c + q
                        add("dve", lambda e, ob=ob, q=q, t=t, h=h, kb_=kb_: e.tensor_scalar(
                            out=oall(t, h * 64, h * 64 + 64), in0=ps[ob][:, q * 65:q * 65 + 64],
                            scalar1=rinv[kb_][:, q:q + 1], scalar2=None, op0=ALU.mult),
                            r=[PS(ob), ("rinv", kb_)], w=[("oall", t, h)])

                for e_ in range(2):
                    for c in (0, 1):
                        moba_attn(e_, c)
                for e_ in range(2):
                    for cc in (2, 3):
                        for ti in range(4):
                            tt = (cc - 2) * 4 + ti
                            add("pe", lambda e, e_=e_, ti=ti, tt=tt: e.transpose(
                                psb[0:32, ti * 128:(ti + 1) * 128], mbtok[:, e_, tt, :], ident[:, :]),
                                r=[("mbtok", e_, tt), "ident"], w=["psb"])
                        add("act", lambda e, e_=e_, cc=cc, qb=qb: e.activation(
                            out=QB(qb[e_])[96:128, cc * 512:(cc + 1) * 512], in_=psb[0:32, 0:512], func=AF.Copy),
                            r=["psb", "qkinit"], w=[(("QB", qb[e_]), "m", cc)])
                for e_ in range(2):
                    for c in (2, 3):
                        moba_attn(e_, c)

        if "nsa" in mix_parts:
            nsa(l)

        if "out" in mix_parts:
            for t in range(NT):
                c = t // 4
                yb = 0
                ork = [("oall", t, h) for h in range(16)]
                alias_w = ["wvn", ("w1s", 0), ("w1s", 1)] if t == 0 else []
                for hf in range(2):
                    add("act", lambda e, t=t, hf=hf: e.activation(
                        out=pt[0][:, :], in_=oall(t, hf * 512, hf * 512 + 512), func=AF.Square,
                        accum_out=ssq[:, hf:hf + 1]),
                        r=ork, w=[("pt", 0), ("ssq", hf)])
                add("act", lambda e: e.activation(out=srt[:, :], in_=ssq[:, :], func=AF.Sqrt,
                                                  bias=epsb[:, 0:1], scale=1.0 / 512),
                    r=[("ssq", 0), ("ssq", 1), "epsb"], w=["srt"])
                add("dve", lambda e: e.reciprocal(out=srt[:, :], in_=srt[:, :]), r=["srt"], w=["srt"])
                for hf in range(2):
                    add("dve", lambda e, t=t, hf=hf, yb=yb: e.scalar_tensor_tensor(
                        out=ytok[yb][:, hf * 512:(hf + 1) * 512], in0=oall(t, hf * 512, hf * 512 + 512),
                        scalar=srt[:, hf:hf + 1], in1=onorm[:, hf * 512:(hf + 1) * 512],
                        op0=ALU.mult, op1=ALU.mult),
                        r=ork + ["srt", "onorm"], w=[("ytok", yb, hf)] + alias_w)
                for k in range(8):
                    add("pe", lambda e, k=k, yb=yb: e.transpose(
                        psb[:, k * 128:(k + 1) * 128], ytok[yb][:, k * 128:(k + 1) * 128], ident[:, :]),
                        r=[("ytok", yb, k // 4), "ident"], w=["psb"])
                for hf in range(2):
                    add("act", lambda e, t=t, hf=hf: e.activation(
                        out=hT[:, 4 * hf:4 * hf + 4, t * 128:(t + 1) * 128],
                        in_=psb[:, 512 * hf:512 * hf + 512].rearrange("p (k t) -> p k t", t=128), func=AF.Copy),
                        r=["psb"], w=[("hT", k, c) for k in range(4 * hf, 4 * hf + 4)])
            for dc in range(8):
                s_ = nxt("wf", WF_SLOTS)
                dma_cast(wf[s_][:], wout_d[l, dc, :, :, :], f"wf{s_}", w=[("wf", s_)])
                for c in range(NCK):
                    cs = slice(c * 512, (c + 1) * 512)
                    pb = nxt("prj", 2)
                    for k in range(8):
                        add("pe", lambda e, pb=pb, s_=s_, k=k, cs=cs: e.matmul(
                            ps[pb][:, :], wf[s_][:, k, :], hT[:, k, cs],
                            start=(k == 0), stop=(k == 7)),
                            r=[("wf", s_), ("hT", k, c)], w=[PS(pb)])
                    add("dve", lambda e, pb=pb, dc=dc, cs=cs: e.tensor_tensor(
                        out=xT[:, dc, cs], in0=ps[pb][:, :], in1=xT[:, dc, cs], op=ALU.add),
                        r=[PS(pb), ("xT", dc, c)], w=[("xT", dc, c)])

    def nsa(l):
        va_all = [(("VA", i), tq) for i in range(2) for tq in range(4)] + [("VAone", 0), ("VAone", 1)]
        for kv in range(2):
            s_ = load_wf(l, 12 + kv)

            def evc(tc, P, pb, kv=kv):
                cs = slice(tc * 512, (tc + 1) * 512)
                for g in range(2):
                    copy_evac("act" if g == 0 else "dve", CIN(g)[64 * kv:64 * kv + 64, cs],
                              P[64 * g:64 * g + 64, :], [PS(pb)], [("CIN", g, kv, tc)] + va_all)
            proj_fm(s_, evc)
        import os
        KSTOP = os.environ.get("KSTOP", "")
        if KSTOP == "n_cin":
            return
        dma_cast(w2s[:], cw2_d[l, :, :, :, :], "w2s", w=["w2s"])
        dma_cast(cpos[:], cpos_d[l, :, :], "cpos", w=["cpos"])
        for kv in range(2):
            add("pe", lambda e, cb=kv: e.matmul(ps[cb][:, 0:512], zeros[:, 0:128], zeros[:, 0:512],
                                                start=True, stop=False),
                r=["zeros"], w=[PS(kv)])
        for piece in range(8):
            s_ = nxt("w1s", 2)
            dma_cast(w1s[s_][:], cw1_d[l, :, piece * 4:(piece + 1) * 4, :], f"w1s{s_}",
                     w=[("w1s", s_)] + ([("pt", 2), ("pt", 3)] if s_ == 1 else []))
            for kv in range(2):
                cb = kv
                rows = slice(64 * kv, 64 * kv + 64)
                for li in range(4):
                    lpos = piece * 4 + li
                    for cc in range(2):
                        for g in range(2):
                            col = (g * 2 + cc) * NCMP
                            add("pe", lambda e, cb=cb, rows=rows, li=li, lpos=lpos, cc=cc, g=g, col=col, s_=s_:
                                e.matmul(ps[cb][:, col:col + NCMP],
                                         w1s[s_][rows, li, cc * 128:(cc + 1) * 128],
                                         CIN(g)[rows, lpos:lpos + 16 * (NCMP - 1) + 1:16],
                                         start=False, stop=False),
                                r=[("w1s", s_)] + [("CIN", g, kv, tc) for tc in range(NCK)], w=[PS(cb)])
                        add("pe", lambda e, cb=cb, rows=rows, li=li, lpos=lpos, cc=cc, s_=s_, piece=piece:
                            e.matmul(ps[cb][:, 508 + cc:509 + cc],
                                     w1s[s_][rows, li, cc * 128:(cc + 1) * 128],
                                     cpos[rows, lpos:lpos + 1],
                                     start=False, stop=(piece == 7 and li == 3 and cc == 1)),
                            r=[("w1s", s_), "cpos"], w=[PS(cb)])
        for kv in range(2):
            cb = kv
            if KSTOP == "n_cmpmm":
                return
            P = ps[cb]
            add("dve", lambda e, P=P: e.tensor_copy(posb[:, 0:2], P[:, 508:510]),
                r=[PS(cb)], w=["posb"])
            for g in range(2):
                for cc in range(2):
                    col = (g * 2 + cc) * NCMP
                    add("act", lambda e, P=P, col=col, cc=cc: e.activation(
                        out=gx[:, col:col + NCMP], in_=P[:, col:col + NCMP], func=AF.Identity,
                        bias=posb[:, cc:cc + 1]),
                        r=[PS(cb), "posb"], w=[("gx", g, cc)])
            gxk = [("gx", g, cc) for g in range(2) for cc in range(2)]
            add("dve", lambda e: e.tensor_tensor(out=gu[:, :], in0=gx[:, :], in1=gx[:, :], op=ALU.mult),
                r=gxk, w=["gu"])
            add("dve", lambda e: e.tensor_scalar(out=gu[:, :], in0=gu[:, :], scalar1=0.044715,
                                                 scalar2=1.0, op0=ALU.mult, op1=ALU.add),
                r=["gu"], w=["gu"])
            add("dve", lambda e: e.tensor_tensor(out=gu[:, :], in0=gu[:, :], in1=gx[:, :], op=ALU.mult),
                r=["gu"] + gxk, w=["gu"])
            add("act", lambda e: e.activation(out=gu[:, :], in_=gu[:, :], func=AF.Sigmoid,
                                              scale=1.5957691216057308),
                r=["gu"], w=["gu"])
            add("dve", lambda e: e.tensor_tensor(out=hid[:, :], in0=gu[:, :], in1=gx[:, :], op=ALU.mult),
                r=["gu"] + gxk, w=["hid"])
            if KSTOP == "n_gelu":
                return
            for g in range(2):
                if kv == 0:
                    for cc in range(2):
                        col = (g * 2 + cc) * NCMP
                        add("pe", lambda e, cc=cc, col=col: e.matmul(
                            ps[6][0:64, 0:NCMP], w2s[:, 0, cc, :], hid[:, col:col + NCMP],
                            start=(cc == 0), stop=(cc == 1)),
                            r=["w2s", "hid"], w=[PS(6)])
                    add("act", lambda e, g=g: e.activation(out=kcT[g][0:64, 0:NCMP], in_=ps[6][0:64, 0:NCMP],
                                                           func=AF.Copy),
                        r=[PS(6)], w=[("kcT", g, "k")])
                else:
                    for cc in range(2):
                        col = (g * 2 + cc) * NCMP
                        add("pe", lambda e, cc=cc, col=col: e.matmul(
                            ps[6][0:NCMP, 128:192], hid[:, col:col + NCMP], w2s[:, 1, cc, :],
                            start=(cc == 0), stop=(cc == 1)),
                            r=["w2s", "hid"], w=[PS(6)])
                    add("dve", lambda e, g=g: e.tensor_copy(vcaug[g][0:NCMP, 0:64], ps[6][0:NCMP, 128:192]),
                        r=[PS(6)], w=[("vcaug", g, "v")])
        if KSTOP == "n_kc":
            return
        cin_all = [("CIN", g, kv, tc) for g in range(2) for kv in range(2) for tc in range(NCK)]
        for i in range(2):
            add("pool", lambda e, i=i: e.memset(VA(i)[:, :, :, 64:65], 1.0), w=[("VAone", i)] + cin_all)
        for c in range(8):
            dma_cast(wvn[:, c, :], winvn_d[l, :, c, :], "wvn", w=["wvn"])
        for t in range(NT):
            pb = nxt("prj", 2)
            for c in range(8):
                add("pe", lambda e, pb=pb, c=c, t=t: e.matmul(
                    ps[pb][:, 0:280], hT[:, c, t * 128:(t + 1) * 128], wvn[:, c, :], start=(c == 0), stop=(c == 7)),
                    r=["wvn", ("hT", c, t // 4)], w=[PS(pb)])
            for g in range(2):
                add("dve", lambda e, pb=pb, g=g, t=t: e.tensor_copy(
                    VA(g)[:, t, :, 0:64],
                    ps[pb][:, 0:256].rearrange("p (b g c) -> p b g c", b=2, g=2, c=64)[:, :, g, :]),
                    r=[PS(pb)], w=[(("VA", g), t // 4)] + (cin_all if t == 0 else []))
            add("act", lambda e, pb=pb, t=t: e.activation(out=gates[:, t, :], in_=ps[pb][:, 256:280],
                                                          func=AF.Sigmoid),
                r=[PS(pb)], w=[("gates", t)])
        add("act", lambda e: e.activation(out=posb[:, 0:1], in_=posb[:, 0:1], func=AF.Copy),
            r=["posb"], w=["posb", ("w1s", 1), ("pt", 2), ("pt", 3)])
        if KSTOP == "n_vn":
            return
        dma_sp(KB(0)[96:128, :], c_eslc_d[:, :], "kbE0", w=[(("KB", 0), "E")])
        for g in range(2):
            for kw in range(2):
                s_ = load_wf(l, 14 + kw)

                def evs(tc, P, pb, kw=kw, g=g):
                    cs = slice(tc * 512, (tc + 1) * 512)
                    copy_evac("act" if kw == 0 else "dve", KB(kw)[0:64, cs], P[64 * g:64 * g + 64, :],
                              [PS(pb)], [(("KB", kw), "k", tc)])
                proj_fm(s_, evs)
            for r_ in range(4):
                dma_sp(QB(r_)[64:68, :], c_qalibi_d[4 * g + r_, :, :], f"qal{r_}", r=["qkinit"],
                       w=[(("QB", r_), "al")])
            for pr in range(2):
                s_ = load_wf(l, 8 + 2 * g + pr)

                def evq(tc, P, pb, pr=pr):
                    cs = slice(tc * 512, (tc + 1) * 512)
                    copy_evac("act", QB(2 * pr)[0:64, cs], P[0:64, :], [PS(pb), "qkinit"],
                              [(("QB", 2 * pr), "q", tc)])
                    copy_evac("dve", QB(2 * pr + 1)[0:64, cs], P[64:128, :], [PS(pb), "qkinit"],
                              [(("QB", 2 * pr + 1), "q", tc)])
                proj_fm(s_, evq)
            if KSTOP == "n_q":
                return
            for c in range(NCK):
                cs = slice(c * 512, (c + 1) * 512)
                if KSTOP == "n_c2start" and c == 2:
                    return
                def cmp_qk(r_):
                    sbk = nxt("st", 4)
                    ST = ps[sbk]
                    add("pe", lambda e, ST=ST, r_=r_, g=g, cs=cs: e.matmul(
                        ST[0:NCMP, :], kcT[g][0:68, 0:NCMP], QB(r_)[0:68, cs], start=True, stop=False),
                        r=[("kcT", g, "k"), ("kcT", g, "al"), (("QB", r_), "q", c), (("QB", r_), "al")], w=[PS(sbk)])
                    add("pe", lambda e, ST=ST, cs=cs: e.matmul(
                        ST[0:NCMP, :], ident[0:NCMP, 0:NCMP], cmpmask[0:NCMP, cs], start=False, stop=True),
                        r=["ident", "cmpmask"], w=[PS(sbk)])
                    pb_ = nxt("pt", 4)
                    add("act", lambda e, ST=ST, pb_=pb_: e.activation(
                        out=pt[pb_][0:NCMP, :], in_=ST[0:NCMP, :], func=AF.Exp, scale=0.125),
                        r=[PS(sbk)], w=[("pt", pb_)])
                    return pb_
                nxt_pb = cmp_qk(0)
                for r_ in range(4):
                    hh = 4 * g + r_
                    pb_ = nxt_pb
                    if r_ < 3:
                        nxt_pb = cmp_qk(r_ + 1)
                    if KSTOP == "n_c1":
                        return
                    ob = 4 + nxt("ob", 2)
                    for q in range(4):
                        add("pe", lambda e, ob=ob, q=q, pb_=pb_, g=g: e.matmul(
                            ps[ob][:, q * 97:(q + 1) * 97], pt[pb_][0:NCMP, q * 128:(q + 1) * 128],
                            vcaug[g][0:NCMP, :], start=True, stop=True),
                            r=[("pt", pb_), ("vcaug", g, "v"), ("vcaug", g, "c")], w=[PS(ob)])
                    if KSTOP == "n_c2":
                        return
                    kb_ = nxt("rs", 4)
                    rinv_of(ob, 97, 64, kb_)
                    add("dve", lambda e, kb_=kb_, hh=hh, c=c: e.tensor_tensor(
                        out=coef[kb_][:, :], in0=rinv[kb_][:, :], in1=gates[:, 4 * c:4 * c + 4, hh * 3 + 0],
                        op=ALU.mult),
                        r=[("rinv", kb_)] + [("gates", 4 * c + q) for q in range(4)], w=[("coef", kb_)])
                    for q in range(4):
                        add("dve", lambda e, ob=ob, q=q, r_=r_, kb_=kb_: e.tensor_scalar(
                            out=oacc[:, q, r_ * 64:(r_ + 1) * 64], in0=ps[ob][:, q * 97:q * 97 + 64],
                            scalar1=coef[kb_][:, q:q + 1], scalar2=None, op0=ALU.mult),
                            r=[PS(ob), ("coef", kb_)], w=[("oacc", q, r_)])
                        if c >= 2:
                            add("dve", lambda e, ob=ob, q=q, kb_=kb_, r_=r_: e.tensor_scalar(
                                out=imp4[r_][:, q, :], in0=ps[ob][:, q * 97 + 65:q * 97 + 97],
                                scalar1=rinv[kb_][:, q:q + 1], scalar2=None, op0=ALU.mult),
                                r=[PS(ob), ("rinv", kb_)], w=[("imp4", r_, q)])
                if KSTOP == "n_cmp" and c == 2:
                    return
                if KSTOP == "n_c3":
                    return
                if c >= 2:
                    i4k = [("imp4", r_, q) for r_ in range(4) for q in range(4)]
                    add("dve", lambda e: e.tensor_tensor(
                        out=score[:, :, :], in0=imp4[0][:, :, :], in1=imp4[1][:, :, :], op=ALU.add),
                        r=i4k, w=["score"])
                    add("dve", lambda e: e.tensor_tensor(
                        out=score2[:, :, :], in0=imp4[2][:, :, :], in1=imp4[3][:, :, :], op=ALU.add),
                        r=i4k, w=[("score2", q) for q in range(4)])
                    add("dve", lambda e: e.tensor_tensor(
                        out=imp4[0][:, :, :], in0=score[:, :, :], in1=score2[:, :, :], op=ALU.add),
                        r=["score"] + [("score2", q) for q in range(4)], w=[("imp4", 0, q) for q in range(4)])
                    add("dve", lambda e, c=c: e.tensor_tensor(
                        out=score[:, :, :], in0=imp4[0][:, :, :], in1=force[:, 4 * (c - 2):4 * (c - 2) + 4, :], op=ALU.add),
                        r=[("imp4", 0, q) for q in range(4)] + ["force"], w=["score"])
                    for q in range(4):
                        add("dve", lambda e, q=q: e.max(out=t8a[:, q, :], in_=score[:, q, :]),
                            r=["score"], w=[("t8a", q)])
                        add("dve", lambda e, q=q: e.match_replace(
                            out=score2[:, q, :], in_to_replace=t8a[:, q, :], in_values=score[:, q, :],
                            imm_value=-1e30),
                            r=["score", ("t8a", q)], w=[("score2", q)])
                        add("dve", lambda e, q=q: e.max(out=t8b[:, q, :], in_=score2[:, q, :]),
                            r=[("score2", q)], w=[("t8b", q)])
                        add("dve", lambda e, q=q: e.tensor_scalar(
                            out=mbn[:, q, :], in0=score[:, q, :], scalar1=t8b[:, q, 7:8], scalar2=-1.0,
                            op0=ALU.is_ge, op1=ALU.add),
                            r=["score", ("t8b", q)], w=[("mbn", q)])
                        add("pe", lambda e, q=q: e.transpose(
                            psb[0:32, q * 128:(q + 1) * 128], mbn[:, q, :], ident[:, :]),
                            r=[("mbn", q), "ident"], w=["psb"])
                    for r_ in range(4):
                        copy_evac("act", QB(r_)[96:128, cs], psb[0:32, 0:512],
                                  ["psb", "qkinit"], [(("QB", r_), "m", c)])
                if KSTOP == "n_topk" and c == 2:
                    return
                for br in (2, 1):
                    if KSTOP == "n_win0" and br == 1:
                        return
                    for r_ in range(4):
                        hh = 4 * g + r_
                        ob = 4 + nxt("ob", 2)
                        if br == 2:
                            attn_chunk(c, QB(r_), ("QB", r_), KB(1), ("KB", 1), 68,
                                       lambda j, g=g: VA(g)[:, j, 1, :], ("VA", g),
                                       lambda i: range(max(0, i - 4), i + 1),
                                       lambda j, i: 0 if j == i else (1 if j == i - 4 else None), ob, False)
                        else:
                            attn_chunk(c, QB(r_), ("QB", r_), KB(0), ("KB", 0), 128,
                                       lambda j, g=g: VA(g)[:, j, 0, :], ("VA", g),
                                       lambda i: range(0, i + 1),
                                       lambda j, i: 0 if j == i else None, ob, True)
                        kb_ = nxt("rs", 4)
                        rinv_of(ob, 65, 64, kb_)
                        add("dve", lambda e, kb_=kb_, hh=hh, c=c, br=br: e.tensor_tensor(
                            out=coef[kb_][:, :], in0=rinv[kb_][:, :], in1=gates[:, 4 * c:4 * c + 4, hh * 3 + br],
                            op=ALU.mult),
                            r=[("rinv", kb_)] + [("gates", 4 * c + q) for q in range(4)], w=[("coef", kb_)])
                        for q in range(4):
                            add("dve", lambda e, ob=ob, q=q, r_=r_, kb_=kb_: e.scalar_tensor_tensor(
                                out=oacc[:, q, r_ * 64:(r_ + 1) * 64], in0=ps[ob][:, q * 65:q * 65 + 64],
                                scalar=coef[kb_][:, q:q + 1], in1=oacc[:, q, r_ * 64:(r_ + 1) * 64],
                                op0=ALU.mult, op1=ALU.add),
                                r=[PS(ob), ("coef", kb_), ("oacc", q, r_)], w=[("oacc", q, r_)])
                if KSTOP == "n_slc0":
                    return
                for q in range(4):
                    t = 4 * c + q
                    add("act", lambda e, q=q, t=t, g=g: e.activation(
                        out=oall(t, 512 + g * 256, 512 + (g + 1) * 256), in_=oacc[:, q, :], func=AF.Copy),
                        r=[("oacc", q, r_) for r_ in range(4)], w=[("oall", t, 8 + 4 * g + r_) for r_ in range(4)])

    for l in range(L):
        if "ffa" in stages:
            ffn(l, 0)
            sc.barrier()
        if "mix" in stages:
            mixer(l)
            sc.barrier()
        if "ffb" in stages:
            ffn(l, 1)
            sc.barrier()
    if final:
        norm_fm(12, final_out=True)
    else:
        for c in range(8):
            dma_sp(outT_d[c * 128:(c + 1) * 128, :], xT[:, c, :], "out", r=[("xT", c, k) for k in range(NCK)])
    for name in dbg:
        if name == "oall":
            dd = nc.dram_tensor("dbg_oall", [128, NT * 1024], BF16, kind="ExternalOutput").ap()
            dma_sp(dd[:, :], arena[:, 0:NT * 1024], "out", r=[])
    sc.barrier()

    sems = {e: es.enter_context(nc.semaphore(f"sem_{e}")) for e in ENGS}
    chsem = {c: es.enter_context(nc.semaphore(f"ch_{c}")) for c in sc.chan_cnt}

    def semof(src):
        return chsem[src[1]] if isinstance(src, tuple) else sems[src]

    def emit(name, e):
        for fn, waits, inc in sc.ops[name]:
            for (src, val) in waits:
                e.wait_ge(semof(src), val)
            if fn is None:
                continue
            ins = fn(e)
            if inc is True:
                ins.then_inc(sems[name], 1)
            elif inc:
                ins.then_inc(chsem[inc[1]], 16)

    with nc.Block() as block:
        @block.tensor
        def _(e):
            emit("pe", e)

        @block.scalar
        def _(e):
            emit("act", e)

        @block.vector
        def _(e):
            emit("dve", e)

        @block.gpsimd
        def _(e):
            emit("pool", e)

        @block.sync
        def _(e):
            emit("sp", e)
    es.close()
    return nc


def _bf(a):
    return np.asarray(a, dtype=np.float32).astype(ml_dtypes.bfloat16)


def make_consts():
    t = np.arange(S)
    c = {}
    c["c_ident"] = _bf(np.eye(128))
    s_ = np.arange(128)[:, None]
    t_ = np.arange(128)[None, :]
    tri = np.zeros((128, 2, 128), np.float32)
    tri[:, 0, :] = np.where(s_ <= t_, 0.0, -BIG)
    tri[:, 1, :] = np.where(s_ > t_, 0.0, -BIG)
    c["c_tri"] = _bf(tri)
    cend = 16 * np.arange(128) + 31
    cm = np.where(cend[:, None] <= t[None, :], 0.0, -BIG)
    cm[127, :] = -BIG
    c["c_cmpmask"] = _bf(cm)
    slopes = 2.0 ** (-np.arange(1, 9))
    qa = np.zeros((8, 4, S), np.float32)
    for h in range(8):
        qa[h, 0] = 8 * slopes[h]
        qa[h, 1] = 8 * slopes[h]
        qa[h, 2] = -8 * slopes[h] * 64 * (t // 64)
        qa[h, 3] = -8 * slopes[h] * (t % 64)
    c["c_qalibi"] = _bf(qa)
    ka = np.stack([64.0 * (t // 64), 1.0 * (t % 64), np.ones(S), np.ones(S)]).astype(np.float32)
    c["c_kalibi"] = _bf(ka)
    kac = np.stack([64.0 * (cend // 64), 1.0 * (cend % 64), np.ones(128), np.ones(128)]).astype(np.float32)
    c["c_kalibic"] = _bf(kac)
    em = np.zeros((32, S), np.float32)
    for n in range(8):
        em[n, n * 256:(n + 1) * 256] = BIG
    c["c_emoba"] = _bf(em)
    esl = np.zeros((32, S), np.float32)
    for j in range(32):
        esl[j, j * 64:(j + 1) * 64] = BIG
    c["c_eslc"] = _bf(esl)
    force = np.zeros((128, 8, 32), np.float32)
    for ti in range(8):
        tt = (8 + ti) * 128 + np.arange(128)
        tb = tt // 64
        jj = np.arange(32)[None, :]
        cand = jj <= tb[:, None]
        forced = (jj == 0) | (jj == tb[:, None]) | (jj == tb[:, None] - 1)
        force[:, ti, :] = np.where(cand, np.where(forced, 1e4, 0.0), -1e30)
    c["c_force"] = force
    mn = np.zeros((128, 8, 8), np.float32)
    for ti in range(8):
        own = (8 + ti) // 2
        for n in range(8):
            mn[:, ti, n] = 0.0 if n < own else (1e30 if n == own else -1e30)
    c["c_mobaneg"] = mn
    va = np.zeros((128, 33), np.float32)
    va[:, 0] = 1.0
    for n in range(NCMP):
        for j in range(32):
            if (16 * n < 64 * j + 64) and (16 * n + 32 > 64 * j):
                va[n, 1 + j] = 1.0
    c["c_vcaug"] = _bf(va)
    return c


def prep_weights(inp):
    f = lambda a: np.ascontiguousarray(np.asarray(a, dtype=np.float32))
    L = L_FULL
    w = {}
    g = np.zeros((128, 13, 8), np.float32)
    for l in range(L):
        for k, nm in enumerate(("ffa_norm", "mix_norm", "ffb_norm")):
            g[:, 3 * l + k, :] = np.asarray(inp[nm])[l].reshape(8, 128).T
    g[:, 12, :] = np.asarray(inp["final_norm"]).reshape(8, 128).T
    w["gains"] = g
    wgu = np.empty((L, 2, NF, 128, 2, 8, 128), np.float32)
    wd = np.empty((L, 2, 2, 8, 128, 11, 128), np.float32)
    ffw = ((inp["ffa_w_gate"], inp["ffa_w_up"], inp["ffa_w_down"]),
           (inp["ffb_w_gate"], inp["ffb_w_up"], inp["ffb_w_down"]))
    for ab in range(2):
        for gi in range(2):
            a = np.asarray(ffw[ab][gi]).reshape(L, 8, 128, NF, 128)
            wgu[:, ab, :, :, gi, :, :] = a.transpose(0, 3, 2, 1, 4)
        a = np.asarray(ffw[ab][2]).reshape(L, 2, 11, 128, 8, 128)
        wd[:, ab] = a.transpose(0, 1, 4, 3, 2, 5)
    w["wgu"] = wgu
    w["wd"] = wd
    win = np.asarray(inp["w_in"]).reshape(L, 8, 128, 2840)
    cols = [0, 128, 256, 384, 512, 640, 768, 896, 1536, 1664, 1792, 1920, 2048, 2176, 2304, 2560]
    w["winf"] = f(np.stack([win[:, :, :, c0:c0 + 128].transpose(0, 2, 1, 3) for c0 in cols], axis=1))
    w["winvm"] = f(np.stack([win[:, :, :, 1024 + 128 * p:1024 + 128 * (p + 1)].transpose(0, 2, 1, 3)
                             for p in range(4)], axis=1))
    vn = np.concatenate([win[:, :, :, 2432:2560], win[:, :, :, 2688:2816], win[:, :, :, 2816:2840]], axis=-1)
    w["winvn"] = f(vn.transpose(0, 2, 1, 3))
    w["wout"] = f(np.asarray(inp["w_out"]).reshape(L, 8, 128, 8, 128).transpose(0, 3, 2, 1, 4))
    w1 = np.stack([np.asarray(inp["cmp_k_w1"]), np.asarray(inp["cmp_v_w1"])], axis=1)
    w1 = w1.reshape(L, 2, 32, 64, 256).transpose(0, 1, 3, 2, 4).reshape(L, 128, 32, 256)
    w["cw1"] = f(w1)
    w2 = np.stack([np.asarray(inp["cmp_k_w2"]), np.asarray(inp["cmp_v_w2"])], axis=1)
    w["cw2"] = f(w2.reshape(L, 2, 2, 128, 64).transpose(0, 3, 1, 2, 4))
    pos = np.stack([np.asarray(inp["cmp_pos_k"]), np.asarray(inp["cmp_pos_v"])], axis=1)
    w["cpos"] = f(pos.transpose(0, 1, 3, 2).reshape(L, 128, 32))
    on = np.concatenate([np.asarray(inp["moba_out_norm"]), np.asarray(inp["nsa_out_norm"])], axis=-1)
    w["onorm"] = f(np.broadcast_to(on[:, None, :], (L, 128, 1024)))
    return w


_NC_CACHE = {}


def kernel(**inputs):
    x = np.asarray(inputs["x"], dtype=np.float32)
    B = x.shape[0]
    shared = {}
    shared.update(make_consts())
    shared.update(prep_weights(inputs))
    if "full" not in _NC_CACHE:
        _NC_CACHE["full"] = build()
    nc = _NC_CACHE["full"]
    in_maps = []
    for b in range(B):
        m = dict(shared)
        m["xT"] = np.ascontiguousarray(x[b].T)
        in_maps.append(m)
    res = run_bass_kernel_spmd(nc, in_maps, core_ids=list(range(B)))
    out = np.stack([np.ascontiguousarray(r["outT"].T) for r in res.results], axis=0)
    return out.astype(np.float32)
```

```python
import bisect
from contextlib import ExitStack

import numpy as np
import ml_dtypes

import concourse.bass as bass
import concourse.mybir as mybir
from concourse.bass_utils import run_bass_kernel_spmd

F32 = mybir.dt.float32
BF16 = mybir.dt.bfloat16
AF = mybir.ActivationFunctionType
ALU = mybir.AluOpType
AX = mybir.AxisListType

S = 2048
D = 1024
DFF = 2816
NF = 22
NT = 16
NCK = 4
L_FULL = 4
EPS = 1e-6
BIG = 32768.0
NCMP = 127

ENGS = ("pe", "act", "dve", "pool", "sp")


class Sched:
    def __init__(self):
        self.ops = {e: [] for e in ENGS}
        self.known = {e: {} for e in ENGS}
        self.hist = {e: [(-1, {})] for e in ENGS}
        self.hist_idx = {e: [-1] for e in ENGS}
        self.marks = {e: [] for e in ENGS}
        self.lastw = {}
        self.readers = {}
        self.chan_cnt = {}
        self.last_compute = {e: -1 for e in ENGS}

    def _snapshot(self, eng):
        idx = len(self.ops[eng])
        self.hist[eng].append((idx, dict(self.known[eng])))
        self.hist_idx[eng].append(idx)

    def _clock(self, eng, idx):
        pos = bisect.bisect_right(self.hist_idx[eng], idx) - 1
        return self.hist[eng][pos][1]

    def _need(self, eng, src, idx, waits):
        kn = self.known[eng]
        if isinstance(src, tuple):
            if kn.get(src, 0) >= idx:
                return False
            tot = self.chan_cnt[src[1]]
            waits.append((src, tot * 16))
            kn[src] = tot
            return True
        if kn.get(src, -1) >= idx:
            return False
        mk = self.marks[src]
        pos = bisect.bisect_left(mk, idx)
        if pos == len(mk):
            mk.append(idx)
            self.ops[src][idx][2] = True
        midx = mk[pos]
        waits.append((src, pos + 1))
        kn[src] = midx
        for s2, v2 in self._clock(src, midx).items():
            if isinstance(s2, tuple):
                if kn.get(s2, 0) < v2:
                    kn[s2] = v2
            elif kn.get(s2, -1) < v2:
                kn[s2] = v2
        return True

    def add(self, eng, fn, r=(), w=(), ch=None):
        deps = []
        lastw = self.lastw
        readers = self.readers
        for b in r:
            d = lastw.get(b)
            if d is not None:
                deps.append(d)
        skip_same = (eng == "pe")
        for b in w:
            d = lastw.get(b)
            if d is not None and not (skip_same and d[0] == eng):
                deps.append(d)
            for d in readers.get(b, ()):
                if not (skip_same and d[0] == eng):
                    deps.append(d)
        waits = []
        changed = False
        for (src, idx) in deps:
            if self._need(eng, src, idx, waits):
                changed = True
        idx = len(self.ops[eng])
        if changed:
            self._snapshot(eng)
        self.ops[eng].append([fn, waits, False])
        if ch is not None:
            self.chan_cnt[ch] = self.chan_cnt.get(ch, 0) + 1
            me = (("ch", ch), self.chan_cnt[ch])
            self.ops[eng][idx][2] = ("ch", ch)
        else:
            me = (eng, idx)
            self.last_compute[eng] = idx
        for b in r:
            readers.setdefault(b, []).append(me)
        for b in w:
            lastw[b] = me
            readers[b] = []
        return idx

    def barrier(self):
        for e in ENGS:
            waits = []
            ch = False
            for f in ENGS:
                if f != e and self.last_compute[f] >= 0:
                    ch |= self._need(e, f, self.last_compute[f], waits)
            for c, n in self.chan_cnt.items():
                ch |= self._need(e, ("ch", c), n, waits)
            if ch:
                self._snapshot(e)
            self.ops[e].append([None, waits, False])
        self.lastw = {}
        self.readers = {}


def build(L=L_FULL, final=True, stages=("ffa", "mix", "ffb"), dbg=(), mix_parts=("moba", "nsa", "out")):
    nc = bass.Bass("TRN2", target_bir_lowering=False)
    sc = Sched()
    es = ExitStack()

    def din(name, shape, dt=F32):
        return nc.dram_tensor(name, list(shape), dt, kind="ExternalInput").ap()

    xT_d = din("xT", [D, S])
    gains_d = din("gains", [128, 13, 8])
    wgu_d = din("wgu", [L_FULL, 2, NF, 128, 2, 8, 128])
    wd_d = din("wd", [L_FULL, 2, 2, 8, 128, 11, 128])
    winf_d = din("winf", [L_FULL, 16, 128, 8, 128])
    winvm_d = din("winvm", [L_FULL, 4, 128, 8, 128])
    winvn_d = din("winvn", [L_FULL, 128, 8, 280])
    wout_d = din("wout", [L_FULL, 8, 128, 8, 128])
    cw1_d = din("cw1", [L_FULL, 128, 32, 256])
    cw2_d = din("cw2", [L_FULL, 128, 2, 2, 64])
    cpos_d = din("cpos", [L_FULL, 128, 32])
    onorm_d = din("onorm", [L_FULL, 128, 1024])
    c_ident_d = din("c_ident", [128, 128], BF16)
    c_tri_d = din("c_tri", [128, 2, 128], BF16)
    c_cmpmask_d = din("c_cmpmask", [128, S], BF16)
    c_qalibi_d = din("c_qalibi", [8, 4, S], BF16)
    c_kalibi_d = din("c_kalibi", [4, S], BF16)
    c_kalibic_d = din("c_kalibic", [4, 128], BF16)
    c_emoba_d = din("c_emoba", [32, S], BF16)
    c_eslc_d = din("c_eslc", [32, S], BF16)
    c_force_d = din("c_force", [128, 8, 32])
    c_mobaneg_d = din("c_mobaneg", [128, 8, 8])
    c_vcaug_d = din("c_vcaug", [128, 33], BF16)
    outT_d = nc.dram_tensor("outT", [D, S], F32, kind="ExternalOutput").ap()
    dbg_d = {}

    DTB = {F32: 4, BF16: 2}
    SB_BASE = 16544
    alloc_state = {"off": SB_BASE, "max": 0}

    def sb(name, shape, dt):
        n = 1
        for s_ in shape[1:]:
            n *= s_
        nbytes = (n * DTB[dt] + 31) // 32 * 32
        off = alloc_state["off"]
        t = nc.alloc_sbuf_tensor_at(name, list(shape), dt, offset=off)
        alloc_state["off"] = off + nbytes
        alloc_state["max"] = max(alloc_state["max"], off + nbytes)
        return t

    xT = sb("xT", [128, 8, S], F32)
    hT = sb("hT", [128, 8, S], BF16)
    NA_OALL = NT * 1024
    NA_VA = 2 * NT * 130
    OFF_VA = NA_OALL
    OFF_QB = OFF_VA + NA_VA
    OFF_KB = OFF_QB + 4 * S
    NA = OFF_KB + 2 * S
    assert NA >= 11 * S
    arena = sb("arena", [128, NA], BF16)
    gains = sb("gains_sb", [128, 13, 8], F32)
    ident = sb("ident", [128, 128], BF16)
    tri = sb("tri", [128, 2, 128], BF16)
    cmpmask = sb("cmpmask", [128, S], BF16)
    ones = sb("ones", [128, 128], BF16)
    zeros = sb("zeros", [128, 512], BF16)
    force = sb("force", [128, 8, 32], F32)
    mobaneg = sb("mobaneg", [128, 8, 8], F32)
    epsb = sb("epsb", [128, 1], F32)
    kcT = [sb(f"kcT{g}", [128, 128], BF16) for g in range(2)]
    vcaug = [sb(f"vcaug{g}", [128, 97], BF16) for g in range(2)]
    ksumf = sb("ksumf", [64, 2, 8], F32)
    ksumb = sb("ksumb", [64, 2, 8], BF16)
    gsb = sb("gsb", [128, 2, 8, 8], F32)
    top8 = sb("top8", [128, 16, 8], F32)
    mbtok = sb("mbtok", [128, 2, 8, 32], BF16)
    rs = [sb(f"rs{i}", [128, 4], F32) for i in range(4)]
    rinv = [sb(f"rinv{i}", [128, 4], F32) for i in range(4)]
    coef = [sb(f"coef{i}", [128, 4], F32) for i in range(4)]
    gates = sb("gates", [128, NT, 24], F32)
    imp = sb("imp", [128, 4, 32], F32)
    impraw = sb("impraw", [128, 4, 32], F32)
    imptmp = sb("imptmp", [128, 4, 32], F32)
    imp3 = sb("imp3", [128, 4, 32], F32)
    imp4 = [imp, impraw, imptmp, imp3]
    score = sb("score", [128, 4, 32], F32)
    score2 = sb("score2", [128, 4, 32], F32)
    t8a = sb("t8a", [128, 4, 8], F32)
    t8b = sb("t8b", [128, 4, 8], F32)
    mbn = sb("mbn", [128, 4, 32], BF16)
    posb = sb("posb", [128, 2], F32)
    ssq = sb("ssq", [128, 32], F32)
    srt = sb("srt", [128, 32], F32)
    off_n = alloc_state["off"]
    sq = [sb(f"sq{i}", [128, 8, 128], BF16) for i in range(2)]
    rstd = [sb(f"rstd{i}", [128, 256], F32) for i in range(2)]
    end_n = alloc_state["off"]
    alloc_state["off"] = off_n
    gx = sb("gx", [128, 508], F32)
    gu = sb("gu", [128, 508], F32)
    hid = sb("hid", [128, 508], BF16)
    assert alloc_state["off"] <= end_n, (alloc_state["off"], end_n)
    alloc_state["off"] = end_n
    off_u = alloc_state["off"]
    WGU_SLOTS = 2
    wgu = [sb(f"wgu{i}", [128, 2, 8, 128], BF16) for i in range(WGU_SLOTS)]
    WD_SLOTS = 2
    wdw = [sb(f"wdw{i}", [128, 11, 128], BF16) for i in range(WD_SLOTS)]
    sg = [sb(f"sg{i}", [128, 512], BF16) for i in range(2)]
    outbuf = [sb(f"outbuf{i}", [128, 256], F32) for i in range(2)]
    alloc_state["off"] = off_u
    WF_SLOTS = 3
    wf = [sb(f"wf{i}", [128, 8, 128], BF16) for i in range(WF_SLOTS)]
    onorm = sb("onorm_sb", [128, 1024], F32)
    w2s = sb("w2s", [128, 2, 2, 64], BF16)
    cpos = sb("cpos_sb", [128, 32], BF16)
    pt = [sb(f"pt{i}", [128, 512], BF16) for i in range(2)]
    oacc = sb("oacc", [128, 4, 256], F32)
    off_a = alloc_state["off"]
    w1s = [sb(f"w1s{i}", [128, 4, 256], BF16) for i in range(2)]
    _save = alloc_state["off"]
    alloc_state["off"] = off_a + 2048
    pt = pt + [sb(f"pt{i}", [128, 512], BF16) for i in (2, 3)]
    alloc_state["off"] = _save
    wvn = sb("wvn", [128, 8, 280], BF16)
    end_a = alloc_state["off"]
    alloc_state["off"] = off_a
    ytok = [sb(f"ytok{i}", [128, 1024], BF16) for i in range(1)]
    alloc_state["off"] = max(alloc_state["off"], end_a)
    assert alloc_state["max"] <= 229376, alloc_state["max"]

    ps = [es.enter_context(nc.psum_tensor(f"ps{i}", [128, 512], F32)) for i in range(7)]
    psb = es.enter_context(nc.psum_tensor("psb", [128, 1024], BF16))

    def PS(i):
        return ("ps", i)

    def oall(tile, a, b):
        return arena[:, tile * 1024 + a: tile * 1024 + b]

    def QB(i):
        return arena[:, OFF_QB + i * S: OFF_QB + (i + 1) * S]

    def KB(i):
        return arena[:, OFF_KB + i * S: OFF_KB + (i + 1) * S]

    def VA(i):
        return arena[:, OFF_VA + i * NT * 130: OFF_VA + (i + 1) * NT * 130].rearrange(
            "p (t h c) -> p t h c", h=2, c=65)

    def CIN(g):
        return arena[:, OFF_VA + g * S: OFF_VA + (g + 1) * S]

    def AT(fi):
        return arena[:, fi * S:(fi + 1) * S]

    add = sc.add
    rot = {}

    def nxt(name, n):
        v = rot.get(name, 0)
        rot[name] = v + 1
        return v % n

    def dma_sp(out, in_, ch, r=(), w=()):
        add("sp", lambda e, o=out, i=in_: e.dma_start(out=o, in_=i), r=r, w=w, ch=ch)

    def dma_cast(out, in_, ch, r=(), w=()):
        add("pool", lambda e, o=out, i=in_: e.dma_start(out=o, in_=i), r=r, w=w, ch=ch)

    for c in range(8):
        dma_sp(xT[:, c, :], xT_d[c * 128:(c + 1) * 128, :], "x", w=[("xT", c, k) for k in range(NCK)])
    dma_sp(gains[:], gains_d[:, :, :], "c0", w=["gains"])
    dma_sp(ident[:], c_ident_d[:, :], "c0", w=["ident"])
    dma_sp(tri[:], c_tri_d[:, :, :], "c0", w=["tri"])
    dma_sp(cmpmask[:], c_cmpmask_d[:, :], "c0", w=["cmpmask"])
    dma_sp(force[:], c_force_d[:, :, :], "c0", w=["force"])
    dma_sp(mobaneg[:], c_mobaneg_d[:, :, :], "c0", w=["mobaneg"])
    for g in range(2):
        dma_sp(kcT[g][64:68, :], c_kalibic_d[:, :], "c0", w=[("kcT", g, "al")])
        dma_sp(vcaug[g][:, 64:97], c_vcaug_d[:, :], "c0", w=[("vcaug", g, "c")])
    add("dve", lambda e: e.memset(ones[:], 1.0), w=["ones"])
    add("dve", lambda e: e.memset(zeros[:], 0.0), w=["zeros"])
    add("dve", lambda e: e.memset(epsb[:], EPS), w=["epsb"])
    add("dve", lambda e: e.memset(mbtok[:], 0.0), w=["mbtok"])

    if dbg:
        add("pool", lambda e: e.memset(arena[:, 0:NA_OALL], 0.0),
            w=[("oall", t, h) for t in range(NT) for h in range(16)])

    def norm_fm(gidx, final_out=False):
        def sqr(qc):
            b = qc % 2
            tc = qc // 4
            cs = slice(qc * 128, (qc + 1) * 128)
            add("act", lambda e, b=b, cs=cs: e.activation(out=sq[b][:], in_=xT[:, :, cs], func=AF.Square),
                r=[("xT", c, tc) for c in range(8)], w=[("sq", b)])
            for c in range(8):
                add("pe", lambda e, b=b, c=c: e.matmul(ps[6 - b][:, 0:128], ones[:, :], sq[b][:, c, :],
                                                       start=(c == 0), stop=(c == 7)),
                    r=[("sq", b), "ones"], w=[PS(6 - b)])
        sqr(0)
        for qc in range(16):
            if qc + 1 < 16:
                sqr(qc + 1)
            b = qc % 2
            tc = qc // 4
            cs = slice(qc * 128, (qc + 1) * 128)
            add("act", lambda e, b=b: e.activation(out=rstd[b][:, 0:128], in_=ps[6 - b][:, 0:128],
                                                   func=AF.Sqrt, bias=epsb[:, 0:1], scale=1.0 / D),
                r=[PS(6 - b), "epsb"], w=[("rstd", b)])
            add("dve", lambda e, b=b: e.reciprocal(out=rstd[b][:, 0:128], in_=rstd[b][:, 0:128]),
                r=[("rstd", b)], w=[("rstd", b)])
            for c in range(8):
                if not final_out:
                    add("dve", lambda e, b=b, c=c, cs=cs: e.scalar_tensor_tensor(
                        out=hT[:, c, cs], in0=xT[:, c, cs], scalar=gains[:, gidx, c:c + 1], in1=rstd[b][:, 0:128],
                        op0=ALU.mult, op1=ALU.mult),
                        r=[("xT", c, tc), ("rstd", b), "gains"], w=[("hT", c, tc)])
                else:
                    ob = nxt("outbuf", 2)
                    add("dve", lambda e, b=b, c=c, cs=cs, ob=ob: e.scalar_tensor_tensor(
                        out=outbuf[ob][:, 0:128], in0=xT[:, c, cs], scalar=gains[:, gidx, c:c + 1],
                        in1=rstd[b][:, 0:128], op0=ALU.mult, op1=ALU.mult),
                        r=[("xT", c, tc), ("rstd", b), "gains"], w=[("outbuf", ob)])
                    dma_sp(outT_d[c * 128:(c + 1) * 128, cs], outbuf[ob][:, 0:128], "out", r=[("outbuf", ob)])

    def ffn(l, ab):
        norm_fm(3 * l + (0 if ab == 0 else 2))
        for half in range(2):
            for fi in range(11):
                f = half * 11 + fi
                s_ = nxt("wgu", WGU_SLOTS)
                for gu_ in range(2):
                    dma_cast(wgu[s_][:, gu_, :, :], wgu_d[l, ab, f, :, gu_, :, :], f"wgu{s_}", w=[("wgu", s_)])
                for tc in range(NCK):
                    cs = slice(tc * 512, (tc + 1) * 512)
                    pb = nxt("ffn_ps", 2)
                    G, U = ps[pb], ps[2 + pb]
                    for gu_, P in ((0, G), (1, U)):
                        for c in range(8):
                            add("pe", lambda e, P=P, s_=s_, gu_=gu_, c=c, cs=cs: e.matmul(
                                P[:, :], wgu[s_][:, gu_, c, :], hT[:, c, cs], start=(c == 0), stop=(c == 7)),
                                r=[("wgu", s_), ("hT", c, tc)], w=[PS(pb + 2 * gu_)])
                    sb_ = nxt("sg", 2)
                    add("act", lambda e, G=G, sb_=sb_: e.activation(out=sg[sb_][:], in_=G[:, :], func=AF.Silu),
                        r=[PS(pb)], w=[("sg", sb_)])
                    add("dve", lambda e, U=U, sb_=sb_, fi=fi, cs=cs: e.tensor_tensor(
                        out=AT(fi)[:, cs], in0=U[:, :], in1=sg[sb_][:], op=ALU.mult),
                        r=[PS(2 + pb), ("sg", sb_)], w=[("AT", fi, tc)])
            for dc in range(8):
                s_ = nxt("wd", WD_SLOTS)
                dma_cast(wdw[s_][:], wd_d[l, ab, half, dc, :, :, :], f"wd{s_}", w=[("wd", s_)])
                for tc in range(NCK):
                    cs = slice(tc * 512, (tc + 1) * 512)
                    pb = 4 + nxt("ffn_pd", 2)
                    for k in range(11):
                        add("pe", lambda e, pb=pb, s_=s_, k=k, cs=cs: e.matmul(
                            ps[pb][:, :], wdw[s_][:, k, :], AT(k)[:, cs], start=(k == 0), stop=(k == 10)),
                            r=[("wd", s_), ("AT", k, tc)], w=[PS(pb)])
                    add("dve", lambda e, pb=pb, dc=dc, cs=cs: e.scalar_tensor_tensor(
                        out=xT[:, dc, cs], in0=ps[pb][:, :], scalar=0.5, in1=xT[:, dc, cs],
                        op0=ALU.mult, op1=ALU.add),
                        r=[PS(pb), ("xT", dc, tc)], w=[("xT", dc, tc)])

    def load_wf(l, blk):
        s_ = nxt("wf", WF_SLOTS)
        dma_cast(wf[s_][:], winf_d[l, blk, :, :, :], f"wf{s_}", w=[("wf", s_)])
        return s_

    def proj_fm(s_, evac):
        for tc in range(NCK):
            cs = slice(tc * 512, (tc + 1) * 512)
            pb = nxt("prj", 2)
            for c in range(8):
                add("pe", lambda e, pb=pb, s_=s_, c=c, cs=cs: e.matmul(
                    ps[pb][:, :], wf[s_][:, c, :], hT[:, c, cs], start=(c == 0), stop=(c == 7)),
                    r=[("wf", s_), ("hT", c, tc)], w=[PS(pb)])
            evac(tc, ps[pb], pb)

    def copy_evac(eng, out, in_, r, w):
        if eng == "act":
            add("act", lambda e: e.activation(out=out, in_=in_, func=AF.Copy), r=r, w=w)
        else:
            add(eng, lambda e: e.tensor_copy(out, in_), r=r, w=w)

    def attn_chunk(c, Qap, qkey, Kap, kkey, krows, vfn, vkey, kts, special, obank, mask_in_q):
        tiles = list(range(4 * c, 4 * c + 4))
        O = ps[obank]
        add("pe", lambda e: e.matmul(O[:, 0:260], zeros[:, 0:128], zeros[:, 0:260], start=True, stop=False),
            r=["zeros"], w=[PS(obank)])
        alljs = sorted(set(j for i in tiles for j in kts(i)))
        lastj = {i: max(kts(i)) for i in tiles}
        qreads = [(qkey, "q", c), (qkey, "al"), "qkinit"]
        if mask_in_q:
            qreads.append((qkey, "m", c))
        def emit_pv(pb_, iset, i_lo, j):
            for i in iset:
                q = i - 4 * c
                add("pe", lambda e, pb_=pb_, i=i, i_lo=i_lo, q=q, j=j, last=(j == lastj[i] and i == tiles[-1]):
                    e.matmul(O[:, q * 65:(q + 1) * 65], pt[pb_][:, (i - i_lo) * 128:(i - i_lo + 1) * 128],
                             vfn(j), start=False, stop=last),
                    r=[("pt", pb_), (vkey, j // 4)], w=[PS(obank)])

        pend = []
        for j in alljs:
            iset = [i for i in tiles if j in kts(i)]
            i_lo, i_hi = min(iset), max(iset)
            N = (i_hi - i_lo + 1) * 128
            sbk = nxt("st", 4)
            ST = ps[sbk]
            specs = [(i, special(j, i)) for i in iset if special(j, i) is not None]
            add("pe", lambda e, ST=ST, j=j, i_lo=i_lo, i_hi=i_hi, N=N, ns=len(specs): e.matmul(
                ST[:, 0:N], Kap[0:krows, j * 128:(j + 1) * 128], Qap[0:krows, i_lo * 128:(i_hi + 1) * 128],
                start=True, stop=(ns == 0)),
                r=qreads + [(kkey, "k", j // 4), (kkey, "al"), (kkey, "E")], w=[PS(sbk)])
            for si, (i, kind) in enumerate(specs):
                add("pe", lambda e, ST=ST, i=i, i_lo=i_lo, kind=kind, last=(si == len(specs) - 1): e.matmul(
                    ST[:, (i - i_lo) * 128:(i - i_lo + 1) * 128], ident[:, :], tri[:, kind, :],
                    start=False, stop=last),
                    r=["ident", "tri"], w=[PS(sbk)])
            pb_ = nxt("pt", 4)
            add("act", lambda e, ST=ST, N=N, pb_=pb_: e.activation(
                out=pt[pb_][:, 0:N], in_=ST[:, 0:N], func=AF.Exp, scale=0.125),
                r=[PS(sbk)], w=[("pt", pb_)])
            pend.append((pb_, iset, i_lo, j))
            if len(pend) > 2:
                emit_pv(*pend.pop(0))
        while pend:
            emit_pv(*pend.pop(0))

    def rinv_of(obank, stride, col, kbuf):
        O = ps[obank]
        add("dve", lambda e: e.tensor_scalar(
            out=rs[kbuf][:, :], in0=O[:, 0:4 * stride].rearrange("p (q c) -> p q c", c=stride)[:, :, col],
            scalar1=1e-30, scalar2=None, op0=ALU.max),
            r=[PS(obank)], w=[("rs", kbuf)])
        add("dve", lambda e: e.reciprocal(out=rinv[kbuf][:, :], in_=rs[kbuf][:, :]),
            r=[("rs", kbuf)], w=[("rinv", kbuf)])

    def mixer(l):
        norm_fm(3 * l + 1)
        dma_sp(onorm[:], onorm_d[l, :, :], "onorm", w=["onorm"])
        add("pool", lambda e: e.memset(arena[:, OFF_QB:NA], 0.0), w=["qkinit"])
        for i in range(2):
            dma_sp(KB(i)[64:68, :], c_kalibi_d[:, :], f"kbal{i}", r=["qkinit"], w=[(("KB", i), "al")])
        for i in range(2):
            dma_sp(KB(i)[96:128, :], c_emoba_d[:, :], f"kbE{i}", r=["qkinit"], w=[(("KB", i), "E")])
        for i in range(2):
            add("pool", lambda e, i=i: e.memset(VA(i)[:, :, :, 64:65], 1.0), w=[("VAone", i)])

        import os
        KSTOP = os.environ.get("KSTOP", "")
        if KSTOP == "init":
            return
        if "moba" in mix_parts:
            for p in range(4 if not KSTOP else 1):
                qb = [0, 1]
                va = p % 2
                for e_ in range(2):
                    h = 2 * p + e_
                    dma_sp(QB(qb[e_])[64:68, :], c_qalibi_d[h, :, :], f"qal{qb[e_]}", r=["qkinit"],
                           w=[(("QB", qb[e_]), "al")])
                s_ = load_wf(l, p)

                def evq(tc, P, pb, qb=qb):
                    cs = slice(tc * 512, (tc + 1) * 512)
                    copy_evac("act", QB(qb[0])[0:64, cs], P[0:64, :], [PS(pb), "qkinit"], [(("QB", qb[0]), "q", tc)])
                    copy_evac("dve", QB(qb[1])[0:64, cs], P[64:128, :], [PS(pb), "qkinit"], [(("QB", qb[1]), "q", tc)])
                proj_fm(s_, evq)
                s_ = load_wf(l, 4 + p)

                def evk(tc, P, pb, qb=qb):
                    for e_ in range(2):
                        for hf in range(2):
                            c0 = tc * 512 + hf * 256
                            add("act", lambda e, e_=e_, hf=hf, c0=c0, P=P, tc=tc: e.activation(
                                out=KB(qb[e_])[0:64, c0:c0 + 256], in_=P[64 * e_:64 * e_ + 64, hf * 256:hf * 256 + 256],
                                func=AF.Copy, accum_out=ksumf[0:64, e_, 2 * tc + hf:2 * tc + hf + 1]),
                                r=[PS(pb), "qkinit"], w=[(("KB", qb[e_]), "k", tc), ("ksumf", e_, tc)])
                proj_fm(s_, evk)
                add("dve", lambda e: e.tensor_copy(ksumb[:, :, :], ksumf[:, :, :]),
                    r=[("ksumf", e_, tc) for e_ in range(2) for tc in range(NCK)], w=["ksumb"])
                if KSTOP == "qk":
                    return
                s_ = nxt("wf", WF_SLOTS)
                dma_cast(wf[s_][:], winvm_d[l, p, :, :, :], f"wf{s_}", w=[("wf", s_)])
                for tq in range(4):
                    pb = nxt("prj", 2)
                    for ti in range(4):
                        t = tq * 4 + ti
                        for c in range(8):
                            add("pe", lambda e, pb=pb, s_=s_, c=c, t=t, ti=ti: e.matmul(
                                ps[pb][:, ti * 128:(ti + 1) * 128], hT[:, c, t * 128:(t + 1) * 128], wf[s_][:, c, :],
                                start=(c == 0), stop=(c == 7)),
                                r=[("wf", s_), ("hT", c, t // 4)], w=[PS(pb)])
                    add("dve", lambda e, pb=pb, tq=tq, va=va: e.tensor_copy(
                        VA(va)[:, 4 * tq:4 * tq + 4, :, 0:64],
                        ps[pb][:, :].rearrange("p (t h c) -> p t h c", h=2, c=64)),
                        r=[PS(pb)], w=[(("VA", va), tq)])
                if KSTOP == "v":
                    return
                for e_ in range(2):
                    for ti in range(8):
                        t = 8 + ti
                        add("pe", lambda e, e_=e_, ti=ti, t=t, qb=qb: e.matmul(
                            ps[6][:, (e_ * 8 + ti) * 8:(e_ * 8 + ti) * 8 + 8],
                            QB(qb[e_])[0:64, t * 128:(t + 1) * 128], ksumb[0:64, e_, :], start=True, stop=True),
                            r=[(("QB", qb[e_]), "q", t // 4), "ksumb"], w=[PS(6)])
                for e_ in range(2):
                    add("dve", lambda e, e_=e_: e.tensor_tensor(
                        out=gsb[:, e_, :, :], in0=ps[6][:, e_ * 64:(e_ + 1) * 64].rearrange("p (t n) -> p t n", n=8),
                        in1=mobaneg[:, :, :], op=ALU.add),
                        r=[PS(6), "mobaneg"], w=[("gsb", e_)])
                    for ti in range(8):
                        add("dve", lambda e, e_=e_, ti=ti: e.max(out=top8[:, e_ * 8 + ti, :], in_=gsb[:, e_, ti, :]),
                            r=[("gsb", e_)], w=[("top8", e_, ti)])
                        add("dve", lambda e, e_=e_, ti=ti: e.tensor_scalar(
                            out=mbtok[:, e_, ti, 0:8], in0=gsb[:, e_, ti, :],
                            scalar1=top8[:, e_ * 8 + ti, 3:4], scalar2=-1.0, op0=ALU.is_ge, op1=ALU.add),
                            r=[("gsb", e_), ("top8", e_, ti)], w=[("mbtok", e_, ti)])
                if KSTOP == "gate":
                    return

                def moba_attn(e_, c):
                    h = 2 * p + e_
                    ob = 4 + nxt("ob", 2)
                    attn_chunk(c, QB(qb[e_]), ("QB", qb[e_]), KB(qb[e_]), ("KB", qb[e_]), 128,
                               lambda j, va=va, e_=e_: VA(va)[:, j, e_, :], ("VA", va),
                               lambda i: range(0, i + 1),
                               lambda j, i: 0 if j == i else None, ob, True)
                    kb_ = nxt("rs", 4)
                    rinv_of(ob, 65, 64, kb_)
                    for q in range(4):
                        t = 4 * c + q
                        add("dve", lambda e, ob=ob, q=q, t=t, h=h, kb_=kb_: e.tensor_scalar(
                            out=oall(t, h * 64, h * 64 + 64), in0=ps[ob][:, q * 65:q * 65 + 64],
                            scalar1=rinv[kb_][:, q:q + 1], scalar2=None, op0=ALU.mult),
                            r=[PS(ob), ("rinv", kb_)], w=[("oall", t, h)])

                for e_ in range(2):
                    for c in (0, 1):
                        moba_attn(e_, c)
                for e_ in range(2):
                    for cc in (2, 3):
                        for ti in range(4):
                            tt = (cc - 2) * 4 + ti
                            add("pe", lambda e, e_=e_, ti=ti, tt=tt: e.transpose(
                                psb[0:32, ti * 128:(ti + 1) * 128], mbtok[:, e_, tt, :], ident[:, :]),
                                r=[("mbtok", e_, tt), "ident"], w=["psb"])
                        add("act", lambda e, e_=e_, cc=cc, qb=qb: e.activation(
                            out=QB(qb[e_])[96:128, cc * 512:(cc + 1) * 512], in_=psb[0:32, 0:512], func=AF.Copy),
                            r=["psb", "qkinit"], w=[(("QB", qb[e_]), "m", cc)])
                for e_ in range(2):
                    for c in (2, 3):
                        moba_attn(e_, c)

        if "nsa" in mix_parts:
            nsa(l)

        if "out" in mix_parts:
            for t in range(NT):
                ork = [("oall", t, h) for h in range(16)]
                for hf in range(2):
                    add("act", lambda e, t=t, hf=hf: e.activation(
                        out=pt[0][:, :], in_=oall(t, hf * 512, hf * 512 + 512), func=AF.Square,
                        accum_out=ssq[:, 2 * t + hf:2 * t + hf + 1]),
                        r=ork, w=[("pt", 0), ("ssq", t)])
            add("act", lambda e: e.activation(out=srt[:, :], in_=ssq[:, :], func=AF.Sqrt,
                                              bias=epsb[:, 0:1], scale=1.0 / 512),
                r=[("ssq", t) for t in range(NT)] + ["epsb"], w=["srt"])
            add("dve", lambda e: e.reciprocal(out=srt[:, :], in_=srt[:, :]), r=["srt"], w=["srt"])
            for t in range(NT):
                c = t // 4
                yb = 0
                ork = [("oall", t, h) for h in range(16)]
                alias_w = ["wvn", ("w1s", 0), ("w1s", 1)] if t == 0 else []
                for hf in range(2):
                    add("dve", lambda e, t=t, hf=hf, yb=yb: e.scalar_tensor_tensor(
                        out=ytok[yb][:, hf * 512:(hf + 1) * 512], in0=oall(t, hf * 512, hf * 512 + 512),
                        scalar=srt[:, 2 * t + hf:2 * t + hf + 1], in1=onorm[:, hf * 512:(hf + 1) * 512],
                        op0=ALU.mult, op1=ALU.mult),
                        r=ork + ["srt", "onorm"], w=[("ytok", yb, hf)] + alias_w)
                for k in range(8):
                    add("pe", lambda e, k=k, yb=yb: e.transpose(
                        psb[:, k * 128:(k + 1) * 128], ytok[yb][:, k * 128:(k + 1) * 128], ident[:, :]),
                        r=[("ytok", yb, k // 4), "ident"], w=["psb"])
                for hf in range(2):
                    add("act", lambda e, t=t, hf=hf: e.activation(
                        out=hT[:, 4 * hf:4 * hf + 4, t * 128:(t + 1) * 128],
                        in_=psb[:, 512 * hf:512 * hf + 512].rearrange("p (k t) -> p k t", t=128), func=AF.Copy),
                        r=["psb"], w=[("hT", k, c) for k in range(4 * hf, 4 * hf + 4)])
            for dc in range(8):
                s_ = nxt("wf", WF_SLOTS)
                dma_cast(wf[s_][:], wout_d[l, dc, :, :, :], f"wf{s_}", w=[("wf", s_)])
                for c in range(NCK):
                    cs = slice(c * 512, (c + 1) * 512)
                    pb = nxt("prj", 2)
                    for k in range(8):
                        add("pe", lambda e, pb=pb, s_=s_, k=k, cs=cs: e.matmul(
                            ps[pb][:, :], wf[s_][:, k, :], hT[:, k, cs],
                            start=(k == 0), stop=(k == 7)),
                            r=[("wf", s_), ("hT", k, c)], w=[PS(pb)])
                    add("dve", lambda e, pb=pb, dc=dc, cs=cs: e.tensor_tensor(
                        out=xT[:, dc, cs], in0=ps[pb][:, :], in1=xT[:, dc, cs], op=ALU.add),
                        r=[PS(pb), ("xT", dc, c)], w=[("xT", dc, c)])

    def nsa(l):
        va_all = [(("VA", i), tq) for i in range(2) for tq in range(4)] + [("VAone", 0), ("VAone", 1)]
        for kv in range(2):
            s_ = load_wf(l, 12 + kv)

            def evc(tc, P, pb, kv=kv):
                cs = slice(tc * 512, (tc + 1) * 512)
                for g in range(2):
                    copy_evac("act" if g == 0 else "dve", CIN(g)[64 * kv:64 * kv + 64, cs],
                              P[64 * g:64 * g + 64, :], [PS(pb)], [("CIN", g, kv, tc)] + va_all)
            proj_fm(s_, evc)
        import os
        KSTOP = os.environ.get("KSTOP", "")
        if KSTOP == "n_cin":
            return
        dma_cast(w2s[:], cw2_d[l, :, :, :, :], "w2s", w=["w2s"])
        dma_cast(cpos[:], cpos_d[l, :, :], "cpos", w=["cpos"])
        for kv in range(2):
            add("pe", lambda e, cb=kv: e.matmul(ps[cb][:, 0:512], zeros[:, 0:128], zeros[:, 0:512],
                                                start=True, stop=False),
                r=["zeros"], w=[PS(kv)])
        for piece in range(8):
            s_ = nxt("w1s", 2)
            dma_cast(w1s[s_][:], cw1_d[l, :, piece * 4:(piece + 1) * 4, :], f"w1s{s_}",
                     w=[("w1s", s_)] + ([("pt", 2), ("pt", 3)] if s_ == 1 else []))
            for kv in range(2):
                cb = kv
                rows = slice(64 * kv, 64 * kv + 64)
                for li in range(4):
                    lpos = piece * 4 + li
                    for cc in range(2):
                        for g in range(2):
                            col = (g * 2 + cc) * NCMP
                            add("pe", lambda e, cb=cb, rows=rows, li=li, lpos=lpos, cc=cc, g=g, col=col, s_=s_:
                                e.matmul(ps[cb][:, col:col + NCMP],
                                         w1s[s_][rows, li, cc * 128:(cc + 1) * 128],
                                         CIN(g)[rows, lpos:lpos + 16 * (NCMP - 1) + 1:16],
                                         start=False, stop=False),
                                r=[("w1s", s_)] + [("CIN", g, kv, tc) for tc in range(NCK)], w=[PS(cb)])
                        add("pe", lambda e, cb=cb, rows=rows, li=li, lpos=lpos, cc=cc, s_=s_, piece=piece:
                            e.matmul(ps[cb][:, 508 + cc:509 + cc],
                                     w1s[s_][rows, li, cc * 128:(cc + 1) * 128],
                                     cpos[rows, lpos:lpos + 1],
                                     start=False, stop=(piece == 7 and li == 3 and cc == 1)),
                            r=[("w1s", s_), "cpos"], w=[PS(cb)])
        for kv in range(2):
            cb = kv
            if KSTOP == "n_cmpmm":
                return
            P = ps[cb]
            add("dve", lambda e, P=P: e.tensor_copy(posb[:, 0:2], P[:, 508:510]),
                r=[PS(cb)], w=["posb"])
            for g in range(2):
                for cc in range(2):
                    col = (g * 2 + cc) * NCMP
                    add("act", lambda e, P=P, col=col, cc=cc: e.activation(
                        out=gx[:, col:col + NCMP], in_=P[:, col:col + NCMP], func=AF.Identity,
                        bias=posb[:, cc:cc + 1]),
                        r=[PS(cb), "posb"], w=[("gx", g, cc)])
            gxk = [("gx", g, cc) for g in range(2) for cc in range(2)]
            add("dve", lambda e: e.tensor_tensor(out=gu[:, :], in0=gx[:, :], in1=gx[:, :], op=ALU.mult),
                r=gxk, w=["gu"])
            add("dve", lambda e: e.tensor_scalar(out=gu[:, :], in0=gu[:, :], scalar1=0.044715,
                                                 scalar2=1.0, op0=ALU.mult, op1=ALU.add),
                r=["gu"], w=["gu"])
            add("dve", lambda e: e.tensor_tensor(out=gu[:, :], in0=gu[:, :], in1=gx[:, :], op=ALU.mult),
                r=["gu"] + gxk, w=["gu"])
            add("act", lambda e: e.activation(out=gu[:, :], in_=gu[:, :], func=AF.Sigmoid,
                                              scale=1.5957691216057308),
                r=["gu"], w=["gu"])
            add("dve", lambda e: e.tensor_tensor(out=hid[:, :], in0=gu[:, :], in1=gx[:, :], op=ALU.mult),
                r=["gu"] + gxk, w=["hid"])
            if KSTOP == "n_gelu":
                return
            for g in range(2):
                if kv == 0:
                    for cc in range(2):
                        col = (g * 2 + cc) * NCMP
                        add("pe", lambda e, cc=cc, col=col: e.matmul(
                            ps[6][0:64, 0:NCMP], w2s[:, 0, cc, :], hid[:, col:col + NCMP],
                            start=(cc == 0), stop=(cc == 1)),
                            r=["w2s", "hid"], w=[PS(6)])
                    add("act", lambda e, g=g: e.activation(out=kcT[g][0:64, 0:NCMP], in_=ps[6][0:64, 0:NCMP],
                                                           func=AF.Copy),
                        r=[PS(6)], w=[("kcT", g, "k")])
                else:
                    for cc in range(2):
                        col = (g * 2 + cc) * NCMP
                        add("pe", lambda e, cc=cc, col=col: e.matmul(
                            ps[6][0:NCMP, 128:192], hid[:, col:col + NCMP], w2s[:, 1, cc, :],
                            start=(cc == 0), stop=(cc == 1)),
                            r=["w2s", "hid"], w=[PS(6)])
                    add("dve", lambda e, g=g: e.tensor_copy(vcaug[g][0:NCMP, 0:64], ps[6][0:NCMP, 128:192]),
                        r=[PS(6)], w=[("vcaug", g, "v")])
        if KSTOP == "n_kc":
            return
        cin_all = [("CIN", g, kv, tc) for g in range(2) for kv in range(2) for tc in range(NCK)]
        for i in range(2):
            add("pool", lambda e, i=i: e.memset(VA(i)[:, :, :, 64:65], 1.0), w=[("VAone", i)] + cin_all)
        for c in range(8):
            dma_cast(wvn[:, c, :], winvn_d[l, :, c, :], "wvn", w=["wvn"])
        for t in range(NT):
            pb = nxt("prj", 2)
            for c in range(8):
                add("pe", lambda e, pb=pb, c=c, t=t: e.matmul(
                    ps[pb][:, 0:280], hT[:, c, t * 128:(t + 1) * 128], wvn[:, c, :], start=(c == 0), stop=(c == 7)),
                    r=["wvn", ("hT", c, t // 4)], w=[PS(pb)])
            for g in range(2):
                add("dve", lambda e, pb=pb, g=g, t=t: e.tensor_copy(
                    VA(g)[:, t, :, 0:64],
                    ps[pb][:, 0:256].rearrange("p (b g c) -> p b g c", b=2, g=2, c=64)[:, :, g, :]),
                    r=[PS(pb)], w=[(("VA", g), t // 4)] + (cin_all if t == 0 else []))
            add("act", lambda e, pb=pb, t=t: e.activation(out=gates[:, t, :], in_=ps[pb][:, 256:280],
                                                          func=AF.Sigmoid),
                r=[PS(pb)], w=[("gates", t)])
        add("act", lambda e: e.activation(out=posb[:, 0:1], in_=posb[:, 0:1], func=AF.Copy),
            r=["posb"], w=["posb", ("w1s", 1), ("pt", 2), ("pt", 3)])
        if KSTOP == "n_vn":
            return
        dma_sp(KB(0)[96:128, :], c_eslc_d[:, :], "kbE0", w=[(("KB", 0), "E")])
        for g in range(2):
            for kw in range(2):
                s_ = load_wf(l, 14 + kw)

                def evs(tc, P, pb, kw=kw, g=g):
                    cs = slice(tc * 512, (tc + 1) * 512)
                    copy_evac("act" if kw == 0 else "dve", KB(kw)[0:64, cs], P[64 * g:64 * g + 64, :],
                              [PS(pb)], [(("KB", kw), "k", tc)])
                proj_fm(s_, evs)
            for r_ in range(4):
                dma_sp(QB(r_)[64:68, :], c_qalibi_d[4 * g + r_, :, :], f"qal{r_}", r=["qkinit"],
                       w=[(("QB", r_), "al")])
            for pr in range(2):
                s_ = load_wf(l, 8 + 2 * g + pr)

                def evq(tc, P, pb, pr=pr):
                    cs = slice(tc * 512, (tc + 1) * 512)
                    copy_evac("act", QB(2 * pr)[0:64, cs], P[0:64, :], [PS(pb), "qkinit"],
                              [(("QB", 2 * pr), "q", tc)])
                    copy_evac("dve", QB(2 * pr + 1)[0:64, cs], P[64:128, :], [PS(pb), "qkinit"],
                              [(("QB", 2 * pr + 1), "q", tc)])
                proj_fm(s_, evq)
            if KSTOP == "n_q":
                return
            for c in range(NCK):
                cs = slice(c * 512, (c + 1) * 512)
                if KSTOP == "n_c2start" and c == 2:
                    return
                def cmp_qk(r_):
                    sbk = nxt("st", 4)
                    ST = ps[sbk]
                    add("pe", lambda e, ST=ST, r_=r_, g=g, cs=cs: e.matmul(
                        ST[0:NCMP, :], kcT[g][0:68, 0:NCMP], QB(r_)[0:68, cs], start=True, stop=False),
                        r=[("kcT", g, "k"), ("kcT", g, "al"), (("QB", r_), "q", c), (("QB", r_), "al")], w=[PS(sbk)])
                    add("pe", lambda e, ST=ST, cs=cs: e.matmul(
                        ST[0:NCMP, :], ident[0:NCMP, 0:NCMP], cmpmask[0:NCMP, cs], start=False, stop=True),
                        r=["ident", "cmpmask"], w=[PS(sbk)])
                    pb_ = nxt("pt", 4)
                    add("act", lambda e, ST=ST, pb_=pb_: e.activation(
                        out=pt[pb_][0:NCMP, :], in_=ST[0:NCMP, :], func=AF.Exp, scale=0.125),
                        r=[PS(sbk)], w=[("pt", pb_)])
                    return pb_
                nxt_pb = cmp_qk(0)
                for r_ in range(4):
                    hh = 4 * g + r_
                    pb_ = nxt_pb
                    if r_ < 3:
                        nxt_pb = cmp_qk(r_ + 1)
                    if KSTOP == "n_c1":
                        return
                    ob = 4 + nxt("ob", 2)
                    for q in range(4):
                        add("pe", lambda e, ob=ob, q=q, pb_=pb_, g=g: e.matmul(
                            ps[ob][:, q * 97:(q + 1) * 97], pt[pb_][0:NCMP, q * 128:(q + 1) * 128],
                            vcaug[g][0:NCMP, :], start=True, stop=True),
                            r=[("pt", pb_), ("vcaug", g, "v"), ("vcaug", g, "c")], w=[PS(ob)])
                    if KSTOP == "n_c2":
                        return
                    kb_ = nxt("rs", 4)
                    rinv_of(ob, 97, 64, kb_)
                    add("dve", lambda e, kb_=kb_, hh=hh, c=c: e.tensor_tensor(
                        out=coef[kb_][:, :], in0=rinv[kb_][:, :], in1=gates[:, 4 * c:4 * c + 4, hh * 3 + 0],
                        op=ALU.mult),
                        r=[("rinv", kb_)] + [("gates", 4 * c + q) for q in range(4)], w=[("coef", kb_)])
                    for q in range(4):
                        add("dve", lambda e, ob=ob, q=q, r_=r_, kb_=kb_: e.tensor_scalar(
                            out=oacc[:, q, r_ * 64:(r_ + 1) * 64], in0=ps[ob][:, q * 97:q * 97 + 64],
                            scalar1=coef[kb_][:, q:q + 1], scalar2=None, op0=ALU.mult),
                            r=[PS(ob), ("coef", kb_)], w=[("oacc", q, r_)])
                        if c >= 2:
                            add("dve", lambda e, ob=ob, q=q, kb_=kb_, r_=r_: e.tensor_scalar(
                                out=imp4[r_][:, q, :], in0=ps[ob][:, q * 97 + 65:q * 97 + 97],
                                scalar1=rinv[kb_][:, q:q + 1], scalar2=None, op0=ALU.mult),
                                r=[PS(ob), ("rinv", kb_)], w=[("imp4", r_, q)])
                if KSTOP == "n_cmp" and c == 2:
                    return
                if KSTOP == "n_c3":
                    return
                if c >= 2:
                    i4k = [("imp4", r_, q) for r_ in range(4) for q in range(4)]
                    add("dve", lambda e: e.tensor_tensor(
                        out=score[:, :, :], in0=imp4[0][:, :, :], in1=imp4[1][:, :, :], op=ALU.add),
                        r=i4k, w=["score"])
                    add("dve", lambda e: e.tensor_tensor(
                        out=score2[:, :, :], in0=imp4[2][:, :, :], in1=imp4[3][:, :, :], op=ALU.add),
                        r=i4k, w=[("score2", q) for q in range(4)])
                    add("dve", lambda e: e.tensor_tensor(
                        out=imp4[0][:, :, :], in0=score[:, :, :], in1=score2[:, :, :], op=ALU.add),
                        r=["score"] + [("score2", q) for q in range(4)], w=[("imp4", 0, q) for q in range(4)])
                    add("dve", lambda e, c=c: e.tensor_tensor(
                        out=score[:, :, :], in0=imp4[0][:, :, :], in1=force[:, 4 * (c - 2):4 * (c - 2) + 4, :], op=ALU.add),
                        r=[("imp4", 0, q) for q in range(4)] + ["force"], w=["score"])
                    for q in range(4):
                        add("dve", lambda e, q=q: e.max(out=t8a[:, q, :], in_=score[:, q, :]),
                            r=["score"], w=[("t8a", q)])
                        add("dve", lambda e, q=q: e.match_replace(
                            out=score2[:, q, :], in_to_replace=t8a[:, q, :], in_values=score[:, q, :],
                            imm_value=-1e30),
                            r=["score", ("t8a", q)], w=[("score2", q)])
                        add("dve", lambda e, q=q: e.max(out=t8b[:, q, :], in_=score2[:, q, :]),
                            r=[("score2", q)], w=[("t8b", q)])
                        add("dve", lambda e, q=q: e.tensor_scalar(
                            out=mbn[:, q, :], in0=score[:, q, :], scalar1=t8b[:, q, 7:8], scalar2=-1.0,
                            op0=ALU.is_ge, op1=ALU.add),
                            r=["score", ("t8b", q)], w=[("mbn", q)])
                if KSTOP == "n_topk" and c == 2:
                    return
                for br in (2, 1):
                    if KSTOP == "n_win0" and br == 1:
                        return
                    if br == 1 and c >= 2:
                        for q in range(4):
                            add("pe", lambda e, q=q: e.transpose(
                                psb[0:32, q * 128:(q + 1) * 128], mbn[:, q, :], ident[:, :]),
                                r=[("mbn", q), "ident"], w=["psb"])
                        for r_ in range(4):
                            copy_evac("act", QB(r_)[96:128, cs], psb[0:32, 0:512],
                                      ["psb", "qkinit"], [(("QB", r_), "m", c)])
                    for r_ in range(4):
                        hh = 4 * g + r_
                        ob = 4 + nxt("ob", 2)
                        if br == 2:
                            attn_chunk(c, QB(r_), ("QB", r_), KB(1), ("KB", 1), 68,
                                       lambda j, g=g: VA(g)[:, j, 1, :], ("VA", g),
                                       lambda i: range(max(0, i - 4), i + 1),
                                       lambda j, i: 0 if j == i else (1 if j == i - 4 else None), ob, False)
                        else:
                            attn_chunk(c, QB(r_), ("QB", r_), KB(0), ("KB", 0), 128,
                                       lambda j, g=g: VA(g)[:, j, 0, :], ("VA", g),
                                       lambda i: range(0, i + 1),
                                       lambda j, i: 0 if j == i else None, ob, True)
                        kb_ = nxt("rs", 4)
                        rinv_of(ob, 65, 64, kb_)
                        add("dve", lambda e, kb_=kb_, hh=hh, c=c, br=br: e.tensor_tensor(
                            out=coef[kb_][:, :], in0=rinv[kb_][:, :], in1=gates[:, 4 * c:4 * c + 4, hh * 3 + br],
                            op=ALU.mult),
                            r=[("rinv", kb_)] + [("gates", 4 * c + q) for q in range(4)], w=[("coef", kb_)])
                        for q in range(4):
                            add("dve", lambda e, ob=ob, q=q, r_=r_, kb_=kb_: e.scalar_tensor_tensor(
                                out=oacc[:, q, r_ * 64:(r_ + 1) * 64], in0=ps[ob][:, q * 65:q * 65 + 64],
                                scalar=coef[kb_][:, q:q + 1], in1=oacc[:, q, r_ * 64:(r_ + 1) * 64],
                                op0=ALU.mult, op1=ALU.add),
                                r=[PS(ob), ("coef", kb_), ("oacc", q, r_)], w=[("oacc", q, r_)])
                if KSTOP == "n_slc0":
                    return
                for q in range(4):
                    t = 4 * c + q
                    add("act", lambda e, q=q, t=t, g=g: e.activation(
                        out=oall(t, 512 + g * 256, 512 + (g + 1) * 256), in_=oacc[:, q, :], func=AF.Copy),
                        r=[("oacc", q, r_) for r_ in range(4)], w=[("oall", t, 8 + 4 * g + r_) for r_ in range(4)])

    for l in range(L):
        if "ffa" in stages:
            ffn(l, 0)
            sc.barrier()
        if "mix" in stages:
            mixer(l)
            sc.barrier()
        if "ffb" in stages:
            ffn(l, 1)
            sc.barrier()
    if final:
        norm_fm(12, final_out=True)
    else:
        for c in range(8):
            dma_sp(outT_d[c * 128:(c + 1) * 128, :], xT[:, c, :], "out", r=[("xT", c, k) for k in range(NCK)])
    for name in dbg:
        if name == "oall":
            dd = nc.dram_tensor("dbg_oall", [128, NT * 1024], BF16, kind="ExternalOutput").ap()
            dma_sp(dd[:, :], arena[:, 0:NT * 1024], "out", r=[])
    sc.barrier()

    sems = {e: es.enter_context(nc.semaphore(f"sem_{e}")) for e in ENGS}
    chsem = {c: es.enter_context(nc.semaphore(f"ch_{c}")) for c in sc.chan_cnt}

    def semof(src):
        return chsem[src[1]] if isinstance(src, tuple) else sems[src]

    def emit(name, e):
        for fn, waits, inc in sc.ops[name]:
            for (src, val) in waits:
                e.wait_ge(semof(src), val)
            if fn is None:
                continue
            ins = fn(e)
            if inc is True:
                ins.then_inc(sems[name], 1)
            elif inc:
                ins.then_inc(chsem[inc[1]], 16)

    with nc.Block() as block:
        @block.tensor
        def _(e):
            emit("pe", e)

        @block.scalar
        def _(e):
            emit("act", e)

        @block.vector
        def _(e):
            emit("dve", e)

        @block.gpsimd
        def _(e):
            emit("pool", e)

        @block.sync
        def _(e):
            emit("sp", e)
    es.close()
    return nc


def _bf(a):
    return np.asarray(a, dtype=np.float32).astype(ml_dtypes.bfloat16)


def make_consts():
    t = np.arange(S)
    c = {}
    c["c_ident"] = _bf(np.eye(128))
    s_ = np.arange(128)[:, None]
    t_ = np.arange(128)[None, :]
    tri = np.zeros((128, 2, 128), np.float32)
    tri[:, 0, :] = np.where(s_ <= t_, 0.0, -BIG)
    tri[:, 1, :] = np.where(s_ > t_, 0.0, -BIG)
    c["c_tri"] = _bf(tri)
    cend = 16 * np.arange(128) + 31
    cm = np.where(cend[:, None] <= t[None, :], 0.0, -BIG)
    cm[127, :] = -BIG
    c["c_cmpmask"] = _bf(cm)
    slopes = 2.0 ** (-np.arange(1, 9))
    qa = np.zeros((8, 4, S), np.float32)
    for h in range(8):
        qa[h, 0] = 8 * slopes[h]
        qa[h, 1] = 8 * slopes[h]
        qa[h, 2] = -8 * slopes[h] * 64 * (t // 64)
        qa[h, 3] = -8 * slopes[h] * (t % 64)
    c["c_qalibi"] = _bf(qa)
    ka = np.stack([64.0 * (t // 64), 1.0 * (t % 64), np.ones(S), np.ones(S)]).astype(np.float32)
    c["c_kalibi"] = _bf(ka)
    kac = np.stack([64.0 * (cend // 64), 1.0 * (cend % 64), np.ones(128), np.ones(128)]).astype(np.float32)
    c["c_kalibic"] = _bf(kac)
    em = np.zeros((32, S), np.float32)
    for n in range(8):
        em[n, n * 256:(n + 1) * 256] = BIG
    c["c_emoba"] = _bf(em)
    esl = np.zeros((32, S), np.float32)
    for j in range(32):
        esl[j, j * 64:(j + 1) * 64] = BIG
    c["c_eslc"] = _bf(esl)
    force = np.zeros((128, 8, 32), np.float32)
    for ti in range(8):
        tt = (8 + ti) * 128 + np.arange(128)
        tb = tt // 64
        jj = np.arange(32)[None, :]
        cand = jj <= tb[:, None]
        forced = (jj == 0) | (jj == tb[:, None]) | (jj == tb[:, None] - 1)
        force[:, ti, :] = np.where(cand, np.where(forced, 1e4, 0.0), -1e30)
    c["c_force"] = force
    mn = np.zeros((128, 8, 8), np.float32)
    for ti in range(8):
        own = (8 + ti) // 2
        for n in range(8):
            mn[:, ti, n] = 0.0 if n < own else (1e30 if n == own else -1e30)
    c["c_mobaneg"] = mn
    va = np.zeros((128, 33), np.float32)
    va[:, 0] = 1.0
    for n in range(NCMP):
        for j in range(32):
            if (16 * n < 64 * j + 64) and (16 * n + 32 > 64 * j):
                va[n, 1 + j] = 1.0
    c["c_vcaug"] = _bf(va)
    return c


def prep_weights(inp):
    f = lambda a: np.ascontiguousarray(np.asarray(a, dtype=np.float32))
    L = L_FULL
    w = {}
    g = np.zeros((128, 13, 8), np.float32)
    for l in range(L):
        for k, nm in enumerate(("ffa_norm", "mix_norm", "ffb_norm")):
            g[:, 3 * l + k, :] = np.asarray(inp[nm])[l].reshape(8, 128).T
    g[:, 12, :] = np.asarray(inp["final_norm"]).reshape(8, 128).T
    w["gains"] = g
    wgu = np.empty((L, 2, NF, 128, 2, 8, 128), np.float32)
    wd = np.empty((L, 2, 2, 8, 128, 11, 128), np.float32)
    ffw = ((inp["ffa_w_gate"], inp["ffa_w_up"], inp["ffa_w_down"]),
           (inp["ffb_w_gate"], inp["ffb_w_up"], inp["ffb_w_down"]))
    for ab in range(2):
        for gi in range(2):
            a = np.asarray(ffw[ab][gi]).reshape(L, 8, 128, NF, 128)
            wgu[:, ab, :, :, gi, :, :] = a.transpose(0, 3, 2, 1, 4)
        a = np.asarray(ffw[ab][2]).reshape(L, 2, 11, 128, 8, 128)
        wd[:, ab] = a.transpose(0, 1, 4, 3, 2, 5)
    w["wgu"] = wgu
    w["wd"] = wd
    win = np.asarray(inp["w_in"]).reshape(L, 8, 128, 2840)
    cols = [0, 128, 256, 384, 512, 640, 768, 896, 1536, 1664, 1792, 1920, 2048, 2176, 2304, 2560]
    w["winf"] = f(np.stack([win[:, :, :, c0:c0 + 128].transpose(0, 2, 1, 3) for c0 in cols], axis=1))
    w["winvm"] = f(np.stack([win[:, :, :, 1024 + 128 * p:1024 + 128 * (p + 1)].transpose(0, 2, 1, 3)
                             for p in range(4)], axis=1))
    vn = np.concatenate([win[:, :, :, 2432:2560], win[:, :, :, 2688:2816], win[:, :, :, 2816:2840]], axis=-1)
    w["winvn"] = f(vn.transpose(0, 2, 1, 3))
    w["wout"] = f(np.asarray(inp["w_out"]).reshape(L, 8, 128, 8, 128).transpose(0, 3, 2, 1, 4))
    w1 = np.stack([np.asarray(inp["cmp_k_w1"]), np.asarray(inp["cmp_v_w1"])], axis=1)
    w1 = w1.reshape(L, 2, 32, 64, 256).transpose(0, 1, 3, 2, 4).reshape(L, 128, 32, 256)
    w["cw1"] = f(w1)
    w2 = np.stack([np.asarray(inp["cmp_k_w2"]), np.asarray(inp["cmp_v_w2"])], axis=1)
    w["cw2"] = f(w2.reshape(L, 2, 2, 128, 64).transpose(0, 3, 1, 2, 4))
    pos = np.stack([np.asarray(inp["cmp_pos_k"]), np.asarray(inp["cmp_pos_v"])], axis=1)
    w["cpos"] = f(pos.transpose(0, 1, 3, 2).reshape(L, 128, 32))
    on = np.concatenate([np.asarray(inp["moba_out_norm"]), np.asarray(inp["nsa_out_norm"])], axis=-1)
    w["onorm"] = f(np.broadcast_to(on[:, None, :], (L, 128, 1024)))
    return w


_NC_CACHE = {}


def kernel(**inputs):
    x = np.asarray(inputs["x"], dtype=np.float32)
    B = x.shape[0]
    shared = {}
    shared.update(make_consts())
    shared.update(prep_weights(inputs))
    if "full" not in _NC_CACHE:
        _NC_CACHE["full"] = build()
    nc = _NC_CACHE["full"]
    in_maps = []
    for b in range(B):
        m = dict(shared)
        m["xT"] = np.ascontiguousarray(x[b].T)
        in_maps.append(m)
    res = run_bass_kernel_spmd(nc, in_maps, core_ids=list(range(B)))
    out = np.stack([np.ascontiguousarray(r["outT"].T) for r in res.results], axis=0)
    return out.astype(np.float32)
```
